# Optimizing a Trainium2 kernel written in Bass

```python
import jax, jax.numpy as jnp
from jax import lax
import numpy as np

D_MODEL = 2048
BATCH = 16
SEQ = 2048
DEPTH = 1

D_MIX = D_MODEL
D_ATTN = D_MIX // 2
D_SSM = D_MIX - D_ATTN
ATTN_HEAD_DIM = 128
N_ATTN_HEADS = D_ATTN // ATTN_HEAD_DIM
ROT_DIM = ATTN_HEAD_DIM // 4
ROPE_THETA = 500000.0
DILATED_PATTERNS = ((128, 1), (512, 4), (2048, 16))
ATTN_BLOCK = 128
SSM_HEAD_DIM = 64
N_SSM_HEADS = D_SSM // SSM_HEAD_DIM
SSM_GROUPS = 8
SSM_STATE = 128
CONV_WIDTH = 4
SSD_CHUNK = 128
D_CONV_CH = D_SSM + 2 * SSM_GROUPS * SSM_STATE
D_IN_PROJ = 3 * D_ATTN + D_SSM + D_CONV_CH + N_SSM_HEADS
N_EXPERT_GROUPS = 8
EXPERTS_PER_GROUP = 8
N_EXPERTS = N_EXPERT_GROUPS * EXPERTS_PER_GROUP
TOP_K_INNER = 2
D_EXPERT = D_MODEL // 4
MOE_BLOCK = 128
EPS = 1e-6

kernel_name = 'hymba_dilated_ssd_hmoe_block'


def rms_norm(x, w):
    xf = x.astype(jnp.float32)
    y = xf * lax.rsqrt(jnp.mean(xf * xf, axis=-1, keepdims=True) + EPS)
    return (y * w.astype(jnp.float32)).astype(x.dtype)


def rope_tables(positions):
    inv_freq = ROPE_THETA ** (-(jnp.arange(0, ROT_DIM, 2, dtype=jnp.float32) / ROT_DIM))
    ang = positions.astype(jnp.float32)[..., None] * inv_freq
    return jnp.cos(ang)[:, :, None, :], jnp.sin(ang)[:, :, None, :]


def partial_rope(t, cos, sin):
    half = ROT_DIM // 2
    tr = t[..., :ROT_DIM].astype(jnp.float32)
    t1, t2 = tr[..., :half], tr[..., half:]
    rot = jnp.concatenate([t1 * cos - t2 * sin, t2 * cos + t1 * sin], axis=-1)
    return jnp.concatenate([rot.astype(t.dtype), t[..., ROT_DIM:]], axis=-1)


def dilated_window_attention(q, k, v, window, dilation):
    b, s, h, e = q.shape
    span = window // dilation
    assert span <= ATTN_BLOCK
    sub_len = s // dilation
    n_blk = -(-sub_len // ATTN_BLOCK)
    pad = n_blk * ATTN_BLOCK - sub_len

    def gather_stride(t):
        t = t.reshape(b, sub_len, dilation, h, e).transpose(0, 2, 1, 3, 4)
        t = jnp.pad(t, ((0, 0), (0, 0), (0, pad), (0, 0), (0, 0)))
        return t.reshape(b, dilation, n_blk, ATTN_BLOCK, h, e)

    def with_prev_block(t):
        prev = jnp.pad(t, ((0, 0), (0, 0), (1, 0), (0, 0), (0, 0), (0, 0)))[:, :, :-1]
        return jnp.concatenate([prev, t], axis=3)

    qb = gather_stride(q)
    kb = with_prev_block(gather_stride(k))
    vb = with_prev_block(gather_stride(v))
    scores = jnp.einsum('bdnqhe,bdnkhe->bdnhqk', qb, kb,
                        preferred_element_type=jnp.float32) * (e ** -0.5)
    qi = jnp.arange(ATTN_BLOCK)[:, None]
    kj = jnp.arange(2 * ATTN_BLOCK)[None, :]
    dist = ATTN_BLOCK + qi - kj
    blk = jnp.arange(n_blk)[:, None, None]
    valid = (dist >= 0) & (dist <= span) & ((blk > 0) | (kj >= ATTN_BLOCK))
    scores = jnp.where(valid[None, None, :, None], scores, -jnp.inf)
    m = jnp.max(scores, axis=-1, keepdims=True)
    p = jnp.exp(scores - m)
    den = jnp.sum(p, axis=-1, keepdims=True)
    inv_den = jnp.swapaxes(1.0 / den[..., 0], -1, -2)[..., None]
    out = jnp.einsum('bdnhqk,bdnkhe->bdnqhe', p, vb.astype(jnp.float32)) * inv_den
    lse = jnp.swapaxes((m + jnp.log(den))[..., 0], -1, -2)
    out = out.reshape(b, dilation, n_blk * ATTN_BLOCK, h, e)[:, :, :sub_len]
    out = out.transpose(0, 2, 1, 3, 4).reshape(b, s, h, e)
    lse = lse.reshape(b, dilation, n_blk * ATTN_BLOCK, h)[:, :, :sub_len]
    lse = lse.transpose(0, 2, 1, 3).reshape(b, s, h)
    return out, lse


def dilated_mixture_attention(q, k, v):
    outs, lses = [], []
    for window, dilation in DILATED_PATTERNS:
        o, l = dilated_window_attention(q, k, v, window, dilation)
        outs.append(o)
        lses.append(l)
    wts = jax.nn.softmax(jnp.stack(lses), axis=0)
    return jnp.sum(wts[..., None] * jnp.stack(outs), axis=0)


def causal_depthwise_conv(x, w, bias):
    ch = x.shape[-1]
    y = lax.conv_general_dilated(x, w[:, None, :].astype(x.dtype), window_strides=(1,),
                                 padding=((CONV_WIDTH - 1, 0),),
                                 dimension_numbers=('NWC', 'WIO', 'NWC'),
                                 feature_group_count=ch)
    return y + bias.astype(x.dtype)


def ssd_chunked(xs, dt, a, bm, cm):
    b, s, h, p = xs.shape
    g, n = bm.shape[2], bm.shape[3]
    r = h // g
    nc = s // SSD_CHUNK
    xdt = (xs * dt[..., None]).reshape(b, nc, SSD_CHUNK, g, r, p)
    acs = jnp.cumsum((dt * a).reshape(b, nc, SSD_CHUNK, g, r), axis=2)
    bc = bm.reshape(b, nc, SSD_CHUNK, g, n)
    cc = cm.reshape(b, nc, SSD_CHUNK, g, n)
    causal = jnp.tril(jnp.ones((SSD_CHUNK, SSD_CHUNK), dtype=bool))
    seg = acs[:, :, :, None] - acs[:, :, None, :]
    decay_in = jnp.exp(jnp.where(causal[:, :, None, None], seg, -jnp.inf))
    cb = jnp.einsum('bclgn,bcsgn->bclsg', cc, bc)
    y_diag = jnp.einsum('bclsg,bclsgr,bcsgrp->bclgrp', cb, decay_in, xdt)
    decay_to_end = jnp.exp(acs[:, :, -1:] - acs)
    states = jnp.einsum('bclgn,bclgr,bclgrp->bcgrpn', bc, decay_to_end, xdt)
    chunk_decay = jnp.exp(acs[:, :, -1])

    def step(carry, inp):
        st, dec = inp
        return carry * dec[..., None, None] + st, carry

    _, prev = lax.scan(step, jnp.zeros_like(states[:, 0]),
                       (jnp.moveaxis(states, 1, 0), jnp.moveaxis(chunk_decay, 1, 0)))
    prev = jnp.moveaxis(prev, 0, 1)
    y_off = jnp.einsum('bclgn,bcgrpn,bclgr->bclgrp', cc, prev, jnp.exp(acs))
    return (y_diag + y_off).reshape(b, s, h, p)


def hybrid_mixer(h, cos, sin, w_in, q_norm_w, k_norm_w, conv_w, conv_b, dt_bias, a_log,
                 d_skip, ssm_norm_w, w_out):
    b, s, _ = h.shape
    proj = jnp.einsum('bsd,de->bse', h, w_in)
    splits = [D_ATTN, 2 * D_ATTN, 3 * D_ATTN, 3 * D_ATTN + D_SSM,
              3 * D_ATTN + D_SSM + D_CONV_CH]
    q, k, v, z, xbc, dt_raw = jnp.split(proj, splits, axis=-1)
    hs = (b, s, N_ATTN_HEADS, ATTN_HEAD_DIM)
    q = partial_rope(rms_norm(q.reshape(hs), q_norm_w), cos, sin)
    k = partial_rope(rms_norm(k.reshape(hs), k_norm_w), cos, sin)
    y_attn = dilated_mixture_attention(q, k, v.reshape(hs)).reshape(b, s, D_ATTN).astype(h.dtype)
    xbc = jax.nn.silu(causal_depthwise_conv(xbc, conv_w, conv_b))
    xs, bm, cm = jnp.split(xbc, [D_SSM, D_SSM + SSM_GROUPS * SSM_STATE], axis=-1)
    dt = jax.nn.softplus(dt_raw.astype(jnp.float32) + dt_bias.astype(jnp.float32))
    a = -jnp.exp(a_log.astype(jnp.float32))
    xs = xs.reshape(b, s, N_SSM_HEADS, SSM_HEAD_DIM).astype(jnp.float32)
    y = ssd_chunked(xs, dt, a,
                    bm.reshape(b, s, SSM_GROUPS, SSM_STATE).astype(jnp.float32),
                    cm.reshape(b, s, SSM_GROUPS, SSM_STATE).astype(jnp.float32))
    y = y + d_skip.astype(jnp.float32)[:, None] * xs
    y = y.reshape(b, s, D_SSM) * jax.nn.silu(z.astype(jnp.float32))
    y = rms_norm(y.reshape(b, s, SSM_GROUPS, D_SSM // SSM_GROUPS),
                 ssm_norm_w.reshape(SSM_GROUPS, D_SSM // SSM_GROUPS))
    y_ssm = y.reshape(b, s, D_SSM).astype(h.dtype)
    return jnp.einsum('bse,ed->bsd', jnp.concatenate([y_attn, y_ssm], axis=-1), w_out)


def hierarchical_moe(h, router_group_w, router_group_b, router_expert_w, router_expert_b,
                     w_gate, w_up, w_down):
    b, s, d = h.shape
    t = b * s
    ht = h.reshape(t, d)
    g_logits = (ht @ router_group_w + router_group_b).astype(jnp.float32)
    g_prob = jax.nn.softmax(g_logits, axis=-1)
    g_gate, g_idx = lax.top_k(g_prob, 1)
    e_logits = (ht @ router_expert_w + router_expert_b).astype(jnp.float32)
    e_logits = e_logits.reshape(t, N_EXPERT_GROUPS, EXPERTS_PER_GROUP)
    e_in = jnp.take_along_axis(e_logits, g_idx[:, :, None], axis=1)[:, 0]
    e_top, e_local = lax.top_k(e_in, TOP_K_INNER)
    e_w = jax.nn.softmax(e_top, axis=-1) * g_gate
    expert_id = g_idx * EXPERTS_PER_GROUP + e_local
    n_assign = t * TOP_K_INNER
    flat_e = expert_id.reshape(-1)
    flat_tok = jnp.arange(n_assign, dtype=jnp.int32) // TOP_K_INNER
    order = jnp.argsort(flat_e)
    se, stok, sw = flat_e[order], flat_tok[order], e_w.reshape(-1)[order]
    counts = jnp.bincount(flat_e, length=N_EXPERTS)
    start = jnp.cumsum(counts) - counts
    padded = (counts + MOE_BLOCK - 1) // MOE_BLOCK * MOE_BLOCK
    pend = jnp.cumsum(padded)
    pstart = pend - padded
    dest = pstart[se] + (jnp.arange(n_assign) - start[se])
    n_blocks = -(-(n_assign + N_EXPERTS * (MOE_BLOCK - 1)) // MOE_BLOCK)
    rows = n_blocks * MOE_BLOCK
    xd = jnp.zeros((rows, d), h.dtype).at[dest].set(ht[stok])
    block_e = jnp.minimum(jnp.searchsorted(pend, jnp.arange(n_blocks) * MOE_BLOCK, side='right'),
                          N_EXPERTS - 1)

    def expert_block(args):
        xb, e = args
        hid = jax.nn.silu(xb @ w_gate[e]) * (xb @ w_up[e])
        return hid @ w_down[e]

    yd = lax.map(expert_block, (xd.reshape(n_blocks, MOE_BLOCK, d), block_e)).reshape(rows, d)
    y_assign = yd[dest].astype(jnp.float32) * sw[:, None]
    out = jax.ops.segment_sum(y_assign, stok, num_segments=t)
    return out.reshape(b, s, d).astype(h.dtype)


def setup_inputs(seed: int = 0) -> dict:
    key = jax.random.key(seed)
    ks = jax.random.split(key, 24)
    f32 = jnp.float32

    def nrm(k, shape, scale):
        return jax.random.normal(k, shape, f32) * scale

    x = nrm(ks[0], (BATCH, SEQ, D_MODEL), 1.0)
    c = nrm(ks[1], (BATCH, D_MODEL), 1.0)
    positions = (jax.random.randint(ks[2], (BATCH, 1), 0, 1024, dtype=jnp.int32)
                 + jnp.arange(SEQ, dtype=jnp.int32)[None, :])
    ada_w = nrm(ks[3], (DEPTH, D_MODEL, 6 * D_MODEL), 0.5 * D_MODEL ** -0.5)
    ada_b = nrm(ks[4], (DEPTH, 6 * D_MODEL), 0.02)
    norm1_w = 1.0 + nrm(ks[5], (DEPTH, D_MODEL), 0.05)
    w_in = nrm(ks[6], (DEPTH, D_MODEL, D_IN_PROJ), D_MODEL ** -0.5)
    q_norm_w = 1.0 + nrm(ks[7], (DEPTH, ATTN_HEAD_DIM), 0.05)
    k_norm_w = 1.0 + nrm(ks[8], (DEPTH, ATTN_HEAD_DIM), 0.05)
    conv_w = nrm(ks[9], (DEPTH, CONV_WIDTH, D_CONV_CH), CONV_WIDTH ** -0.5)
    conv_b = nrm(ks[10], (DEPTH, D_CONV_CH), 0.02)
    dt0 = jnp.exp(jax.random.uniform(ks[11], (DEPTH, N_SSM_HEADS), f32,
                                     float(np.log(1e-3)), float(np.log(1e-1))))
    dt_bias = dt0 + jnp.log(-jnp.expm1(-dt0))
    a_log = jnp.log(jax.random.uniform(ks[12], (DEPTH, N_SSM_HEADS), f32, 1.0, 16.0))
    d_skip = 1.0 + nrm(ks[13], (DEPTH, N_SSM_HEADS), 0.1)
    ssm_norm_w = 1.0 + nrm(ks[14], (DEPTH, D_SSM), 0.05)
    w_out = nrm(ks[15], (DEPTH, D_MIX, D_MODEL), D_MIX ** -0.5)
    norm2_w = 1.0 + nrm(ks[16], (DEPTH, D_MODEL), 0.05)
    router_group_w = nrm(ks[17], (DEPTH, D_MODEL, N_EXPERT_GROUPS), D_MODEL ** -0.5)
    router_group_b = nrm(ks[18], (DEPTH, N_EXPERT_GROUPS), 0.01)
    router_expert_w = nrm(ks[19], (DEPTH, D_MODEL, N_EXPERTS), D_MODEL ** -0.5)
    router_expert_b = nrm(ks[20], (DEPTH, N_EXPERTS), 0.01)
    w_gate = nrm(ks[21], (DEPTH, N_EXPERTS, D_MODEL, D_EXPERT), D_MODEL ** -0.5)
    w_up = nrm(ks[22], (DEPTH, N_EXPERTS, D_MODEL, D_EXPERT), D_MODEL ** -0.5)
    w_down = nrm(ks[23], (DEPTH, N_EXPERTS, D_EXPERT, D_MODEL), D_EXPERT ** -0.5)
    return {'x': x, 'c': c, 'positions': positions, 'ada_w': ada_w, 'ada_b': ada_b,
            'norm1_w': norm1_w, 'w_in': w_in, 'q_norm_w': q_norm_w, 'k_norm_w': k_norm_w,
            'conv_w': conv_w, 'conv_b': conv_b, 'dt_bias': dt_bias, 'a_log': a_log,
            'd_skip': d_skip, 'ssm_norm_w': ssm_norm_w, 'w_out': w_out, 'norm2_w': norm2_w,
            'router_group_w': router_group_w, 'router_group_b': router_group_b,
            'router_expert_w': router_expert_w, 'router_expert_b': router_expert_b,
            'w_gate': w_gate, 'w_up': w_up, 'w_down': w_down}


def reference(x, c, positions, ada_w, ada_b, norm1_w, w_in, q_norm_w, k_norm_w, conv_w, conv_b,
              dt_bias, a_log, d_skip, ssm_norm_w, w_out, norm2_w, router_group_w, router_group_b,
              router_expert_w, router_expert_b, w_gate, w_up, w_down):
    cos, sin = rope_tables(positions)
    c_act = jax.nn.silu(c)
    for l in range(DEPTH):
        mod = jnp.einsum('bd,de->be', c_act, ada_w[l]) + ada_b[l]
        sh1, sc1, g1, sh2, sc2, g2 = jnp.split(mod[:, None, :], 6, axis=-1)
        h = rms_norm(x, norm1_w[l]) * (1.0 + sc1) + sh1
        x = x + g1 * hybrid_mixer(h, cos, sin, w_in[l], q_norm_w[l], k_norm_w[l], conv_w[l],
                                  conv_b[l], dt_bias[l], a_log[l], d_skip[l], ssm_norm_w[l],
                                  w_out[l])
        h = rms_norm(x, norm2_w[l]) * (1.0 + sc2) + sh2
        x = x + g2 * hierarchical_moe(h, router_group_w[l], router_group_b[l], router_expert_w[l],
                                      router_expert_b[l], w_gate[l], w_up[l], w_down[l])
    return x
```

```python
import contextlib
import math
import numpy as np
import concourse.bass as bass
import concourse.mybir as mybir
from concourse.bass_utils import run_bass_kernel_spmd

F32 = mybir.dt.float32
BF16 = mybir.dt.bfloat16
I32 = mybir.dt.int32
AF = mybir.ActivationFunctionType
ALU = mybir.AluOpType
AX = mybir.AxisListType

NCORES = 8
D = 2048
S = 2048
NB = 2
NT = S // 128
DIN = 7184
NE = 64
PADR = 256
NSB = 96
NBLK = NSB
NROWS = NSB * PADR
DE = 512
EPS = 1e-6
NEG = -30000.0


class Buf:
    __slots__ = ("name", "w", "r", "pre", "sems")

    def __init__(self, name):
        self.name = name
        self.w = {}
        self.r = {}
        self.pre = {}
        self.sems = {}


def _add(d, tok):
    k = id(tok[0])
    o = d.get(k)
    if o is None or o[1] < tok[1]:
        d[k] = tok


class Trk:
    def __init__(self, nc, same_engine_sync=True):
        self.nc = nc
        self.same = same_engine_sync
        self.eng = {}
        for nm, h in (("pe", nc.tensor), ("act", nc.scalar), ("dve", nc.vector),
                      ("pool", nc.gpsimd), ("sp", nc.sync)):
            self.eng[nm] = dict(h=h, sem=nc.alloc_semaphore(name="s_" + nm), cnt=0, waited={}, name=nm)
        self.bufs = {}
        self.bg = set()
        self.dmasems = {}
        self.nins = 0

    def buf(self, name):
        b = self.bufs.get(name)
        if b is None:
            b = Buf(name)
            self.bufs[name] = b
        return b

    def _wait(self, e, deps):
        for sem, val in deps:
            if sem is e["sem"] and (not self.same or e["name"] == "pe"):
                continue
            k = id(sem)
            db = self.dmasems.get(k)
            if db is not None:
                val = db[1]
            if e["waited"].get(k, 0) >= val:
                continue
            e["h"].wait_ge(sem, val)
            e["waited"][k] = val

    def _deps(self, reads, writes, pwrites):
        deps = []
        for b in reads:
            deps.extend(b.w.values())
        for b in writes:
            deps.extend(b.w.values())
            deps.extend(b.r.values())
        for b in pwrites:
            if b.r:
                pre = dict(b.w)
                for t in b.r.values():
                    _add(pre, t)
                b.pre, b.w, b.r = pre, {}, {}
            deps.extend(b.pre.values())
        return deps

    def _commit(self, tok, reads, writes, pwrites):
        for b in writes:
            b.w = {id(tok[0]): tok}
            b.r = {}
            b.pre = {}
        for b in pwrites:
            _add(b.w, tok)
        for b in reads:
            _add(b.r, tok)

    def op(self, eng, fn, reads=(), writes=(), pwrites=()):
        e = self.eng[eng]
        self._wait(e, self._deps(reads, writes, pwrites))
        ins = fn(e["h"])
        e["cnt"] += 1
        ins.then_inc(e["sem"], 1)
        self.nins += 1
        self._commit((e["sem"], e["cnt"]), reads, writes, pwrites)

    def dma(self, q, out, in_, dst, src, indirect=None, extra_reads=(), partial=True, **kw):
        e = self.eng[q]
        reads = [src] + list(extra_reads)
        writes, pwrites = ([], [dst]) if partial else ([dst], [])
        self._wait(e, self._deps(reads, writes, pwrites))
        kind = "sw" if q == "pool" else "hw"
        ent = dst.sems.get(kind)
        if ent is None:
            ent = [self.nc.alloc_semaphore(name="d%s_%s" % (kind, dst.name)), 0]
            dst.sems[kind] = ent
            self.dmasems[id(ent[0])] = ent
        if indirect is not None:
            ins = e["h"].indirect_dma_start(out, indirect.get("out"), in_, indirect.get("in"), **kw)
        else:
            ins = e["h"].dma_start(out=out, in_=in_, **kw)
        ent[1] += 16
        ins.then_inc(ent[0], 16)
        self.nins += 1
        self._commit((ent[0], ent[1]), reads, writes, pwrites)

    def barrier(self, final=False):
        bgs = set() if final else {id(ent[0]) for n in self.bg for ent in self.buf(n).sems.values()}
        toks = [(e["sem"], e["cnt"]) for e in self.eng.values() if e["cnt"] > 0]
        toks += [(ent[0], ent[1]) for ent in self.dmasems.values() if ent[1] > 0 and id(ent[0]) not in bgs]
        for e in self.eng.values():
            self._wait(e, toks)
        for b in self.bufs.values():
            if final or b.name not in self.bg:
                b.w, b.r, b.pre = {}, {}, {}

    def wait_bufs(self, q, bufs):
        e = self.eng[q]
        deps = []
        for b in bufs:
            deps.extend(b.w.values())
        self._wait(e, deps)


class K:
    def __init__(self, stop_after=None, debug=False):
        self.stop_after = stop_after
        self.debug = debug
        nc = self.nc = bass.Bass("TRN2", target_bir_lowering=False)
        self.T = Trk(nc)
        self.din = {}
        self.dbg = {}

    def inp(self, name, shape, dt=F32):
        t = self.nc.dram_tensor(name, list(shape), dt, kind="ExternalInput").ap()
        self.din[name] = t
        self.T.buf(name)
        return t

    def scratch(self, name, shape, dt):
        t = self.nc.dram_tensor(name, list(shape), dt, kind="Internal").ap()
        self.T.buf(name)
        return t

    def outp(self, name, shape, dt=F32):
        t = self.nc.dram_tensor(name, list(shape), dt, kind="ExternalOutput").ap()
        self.T.buf(name)
        return t

    def sb(self, es, name, shape, dt=F32):
        self.uid = getattr(self, "uid", 0) + 1
        h = es.enter_context(self.nc.sbuf_tensor("%s_u%d" % (name, self.uid), list(shape), dt))
        return h.ap()

    def ps(self, es, name, shape, dt=F32):
        self.uid = getattr(self, "uid", 0) + 1
        h = es.enter_context(self.nc.psum_tensor("%s_u%d" % (name, self.uid), list(shape), dt))
        return h.ap()

    def B(self, *names):
        return [self.T.buf(n) for n in names]

    def convert_some(self, k):
        for _ in range(k):
            if self.cq:
                d_, s_, dn, sn = self.cq.pop()
                self.T.dma("pool", d_, s_, self.T.buf(dn), self.T.buf(sn))

    def dump(self, name, src_ap, src_buf, shape, dt=F32):
        o = self.outp("dbg_" + name, shape, dt)
        self.T.dma("sp", o, src_ap, self.T.buf("dbg_" + name), src_buf)
        self.dbg[name] = o

    def declare(self):
        nc = self.nc
        i = self.inp
        self.x = i("x", [NB * S, D])
        self.cT = i("cT", [128, 16, NB])
        self.pos = i("pos", [128, NB * NT], I32)
        self.ada_w = i("ada_w", [D, 6 * D])
        self.ada_bT = i("ada_bT", [128, 96])
        self.n1wT = i("n1wT", [128, 16])
        self.w_in = i("w_in", [D, DIN])
        self.qkw = i("qkw", [2, 128])
        self.cwT = i("cwT", [128, 24, 4])
        self.cbT = i("cbT", [128, 24])
        self.dtb = i("dtb", [16])
        self.alog = i("alog", [16])
        self.dskip = i("dskip", [16])
        self.ssm_nw = i("ssm_nw", [1024])
        self.w_out = i("w_out", [D, D])
        self.n2w = i("n2w", [D])
        self.wr = i("wr", [D, 72])
        self.br = i("br", [72])
        self.w_gate = i("w_gate", [NE * 128, 16 * DE])
        self.w_up = i("w_up", [NE * 128, 16 * DE])
        self.w_down = i("w_down", [NE * 128, 4 * D])
        self.c_ident = i("c_ident", [128, 128])
        self.c_tbx = i("c_tbx", [128, 19 * 128])
        self.c_U = i("c_U", [128, 128])
        self.c_SB = i("c_SB", [128, 128])
        self.c_invf = i("c_invf", [16])
        self.c_bst = i("c_bst", [NSB])
        self.c_pidx = i("c_pidx", [128, 1])
        self.out = self.outp("out", [NB * S, D])
        s = self.scratch
        self.mod_d = s("mod_d", [NB, 6 * D], F32)
        self.qT_d = s("qT_d", [NB, 8, 128, S], BF16)
        self.kT_d = s("kT_d", [NB, 8, 128, S], BF16)
        self.v_d = s("v_d", [NB, S, 1024], BF16)
        self.z_d = s("z_d", [NB, S, 1024], BF16)
        self.xs_d = s("xs_d", [NB, S, 1024], BF16)
        self.bt_d = s("bt_d", [NB, S, 1024], BF16)
        self.BT_d = s("BT_d", [NB, 8, 128, S], BF16)
        self.CT_d = s("CT_d", [NB, 8, 128, S], BF16)
        self.dt_d = s("dt_d", [NB, S, 16], F32)
        self.yT_d = s("yT_d", [NB, 16, 128, S], BF16)
        self.x1_d = s("x1_d", [NB * S, D], F32)
        self.h2_d = s("h2_d", [NB * S, D], BF16)
        self.wgb_d = s("wgb_d", [NE * 128, 16 * DE], BF16)
        self.wub_d = s("wub_d", [NE * 128, 16 * DE], BF16)
        self.wdb_d = s("wdb_d", [NE * 128, 4 * D], BF16)
        self.xd_d = s("xd_d", [NROWS, D], BF16)
        self.yd_d = s("yd_d", [NROWS, D], BF16)
        self.pes = contextlib.ExitStack()
        es = self.pes
        self.ident_f = self.sb(es, "ident_f", [128, 128], F32)
        self.ident_b = self.sb(es, "ident_b", [128, 128], BF16)
        self.A1T = self.sb(es, "A1T", [128, NB, 16], F32)
        self.S1T = self.sb(es, "S1T", [128, NB, 16], F32)
        self.cos_t = self.sb(es, "cos_t", [128, NB * NT, 16], F32)
        self.sin_t = self.sb(es, "sin_t", [128, NB * NT, 16], F32)
        self.dest = self.sb(es, "dest", [128, NB * NT, 2], I32)
        self.swt = self.sb(es, "swt", [128, NB * NT, 2], F32)
        self.OHs = self.sb(es, "OHs", [128, NB * NT, 2, NE], BF16)
        self.rks = self.sb(es, "rks", [128, NB * NT, 2], F32)
        self.runtot = self.sb(es, "runtot", [128, NE], F32)
        self.widx = self.sb(es, "widx", [128, NBLK], I32)

    def p0(self):
        nc, T = self.nc, self.T
        B = self.B
        with contextlib.ExitStack() as es:
            cT = self.sb(es, "p0_cT", [128, 16, NB])
            cact = self.sb(es, "p0_cact", [128, 16, NB], BF16)
            abT = self.sb(es, "p0_abT", [128, 96])
            n1wT = self.sb(es, "p0_n1wT", [128, 16])
            modT = self.sb(es, "p0_modT", [128, NB, 96])
            modrow = self.sb(es, "p0_modrow", [96, 128])
            wb = [self.sb(es, "p0_wb%d" % j, [128, 16, 512], BF16) for j in range(2)]
            posi = self.sb(es, "p0_posi", [128, NB * NT], I32)
            posf = self.sb(es, "p0_posf", [128, NB * NT])
            invf = self.sb(es, "p0_invf", [128, 16])
            ang = self.sb(es, "p0_ang", [128, NB * NT, 16])
            a1 = self.sb(es, "p0_a1", [128, NB * NT, 16])
            modps = self.ps(es, "p0_modps", [128, NB, 96])
            tps = self.ps(es, "p0_tps", [96, 128])
            T.dma("sp", self.ident_f, self.c_ident, *B("ident_f", "c_ident"))
            T.dma("pool", self.ident_b, self.c_ident, *B("ident_b", "c_ident"))
            T.dma("sp", cT, self.cT, *B("p0_cT", "cT"))
            T.dma("sp", abT, self.ada_bT, *B("p0_abT", "ada_bT"))
            T.dma("sp", n1wT, self.n1wT, *B("p0_n1wT", "n1wT"))
            T.dma("sp", posi, self.pos, *B("p0_posi", "pos"))
            T.dma("sp", invf, self.c_invf.partition_broadcast(128), *B("p0_invf", "c_invf"))
            T.op("act", lambda e: e.activation(out=cact, in_=cT, func=AF.Silu), B("p0_cT"), B("p0_cact"))
            T.op("dve", lambda e: e.tensor_copy(out=posf, in_=posi), B("p0_posi"), B("p0_posf"))
            sh3 = [128, NB * NT, 16]
            T.op("dve", lambda e: e.tensor_tensor(out=ang, in0=posf.unsqueeze(2).to_broadcast(sh3),
                                                  in1=invf.unsqueeze(1).to_broadcast(sh3), op=ALU.mult),
                 B("p0_posf", "p0_invf"), B("p0_ang"))
            ui = self.sb(es, "p0_ui", [128, NB * NT, 16], I32)
            uf = self.sb(es, "p0_uf", [128, NB * NT, 16])
            for tab, off, tn in ((self.sin_t, 0.5, "sin_t"), (self.cos_t, 0.75, "cos_t")):
                T.op("dve", lambda e, off=off: e.tensor_scalar(out=a1, in0=ang, scalar1=1.0 / (2.0 * math.pi), scalar2=off,
                                                               op0=ALU.mult, op1=ALU.add), B("p0_ang"), B("p0_a1"))
                T.op("dve", lambda e: e.tensor_copy(out=ui, in_=a1), B("p0_a1"), B("p0_ui"))
                T.op("dve", lambda e: e.tensor_copy(out=uf, in_=ui), B("p0_ui"), B("p0_uf"))
                T.op("dve", lambda e: e.tensor_tensor(out=a1, in0=a1, in1=uf, op=ALU.subtract), B("p0_a1", "p0_uf"), B("p0_a1"))
                T.op("dve", lambda e: e.tensor_scalar(out=uf, in0=a1, scalar1=0.0, scalar2=None, op0=ALU.is_lt),
                     B("p0_a1"), B("p0_uf"))
                T.op("dve", lambda e: e.tensor_tensor(out=a1, in0=a1, in1=uf, op=ALU.add), B("p0_a1", "p0_uf"), B("p0_a1"))
                T.op("act", lambda e, tab=tab: e.activation(out=tab, in_=a1, func=AF.Sin, scale=2.0 * math.pi, bias=-math.pi),
                     B("p0_a1"), B(tn))
            wsrc = self.ada_w.rearrange("(c p) n -> p c n", p=128)
            for n in range(24):
                w = wb[n % 2]
                wn = "p0_wb%d" % (n % 2)
                T.dma("pool", w, wsrc[:, :, n * 512:(n + 1) * 512], *B(wn, "ada_w"), partial=False)
                for m in range(4):
                    for k in range(16):
                        T.op("pe", lambda e, w=w, m=m, k=k, n=n: e.matmul(
                            modps[:, :, n * 4 + m], lhsT=w[:, k, m * 128:(m + 1) * 128], rhs=cact[:, k, :],
                            start=(k == 0), stop=(k == 15)), B(wn, "p0_cact"), (), B("p0_modps"))
            T.op("dve", lambda e: e.tensor_tensor(out=modT, in0=modps, in1=abT.unsqueeze(1).to_broadcast([128, NB, 96]),
                                                  op=ALU.add), B("p0_modps", "p0_abT"), B("p0_modT"))
            for b in range(NB):
                T.op("dve", lambda e, b=b: e.scalar_tensor_tensor(out=self.A1T[:, b, :], in0=modT[:, b, 16:32], scalar=1.0,
                                                                  in1=n1wT, op0=ALU.add, op1=ALU.mult),
                     B("p0_modT", "p0_n1wT"), (), B("A1T"))
                T.op("dve", lambda e, b=b: e.tensor_copy(out=self.S1T[:, b, :], in_=modT[:, b, 0:16]),
                     B("p0_modT"), (), B("S1T"))
                T.op("pe", lambda e, b=b: e.transpose(out=tps, in_=modT[:, b, :], identity=self.ident_f),
                     B("p0_modT", "ident_f"), B("p0_tps"))
                T.op("dve", lambda e: e.tensor_copy(out=modrow, in_=tps), B("p0_tps"), B("p0_modrow"))
                T.dma("sp", self.mod_d[b].rearrange("(j p) -> j p", p=128), modrow, *B("mod_d", "p0_modrow"))
            T.barrier()

    def p1(self):
        T, B = self.T, self.B
        zt = self.sb(self.pes, "zt", [128, 2048], BF16)
        T.op("pool", lambda e: e.memset(zt, 0.0), (), B("zt"))
        per = NROWS // 128 * D
        xdv = self.xd_d.rearrange("(p r) d -> p (r d)", p=128)
        self.zq = [(xdv[:, q * 2048:(q + 1) * 2048], zt) for q in range(per // 2048)]
        self.cq = []
        T.bg = {"wgb_d", "wub_d", "wdb_d"}
        for e_ in range(NE):
            for dst, src, dn, sn in ((self.wgb_d, self.w_gate, "wgb_d", "w_gate"), (self.wub_d, self.w_up, "wub_d", "w_up"),
                                     (self.wdb_d, self.w_down, "wdb_d", "w_down")):
                self.cq.append((dst[e_ * 128:(e_ + 1) * 128, :], src[e_ * 128:(e_ + 1) * 128, :], dn, sn))
        self.cq.reverse()
        for b in range(NB):
            self.p1_batch(b)

    def p1_batch(self, b):
        nc, T, B = self.nc, self.T, self.B
        with contextlib.ExitStack() as es:
            sb, ps = (lambda *a, **k: self.sb(es, *a, **k)), (lambda *a, **k: self.ps(es, *a, **k))
            hT = sb("p1_hT", [128, 16, S], BF16)
            xt = [sb("p1_xt%d" % j, [128, D]) for j in range(2)]
            xn = [sb("p1_xn%d" % j, [128, D], BF16) for j in range(2)]
            junk = sb("p1_junk", [128, D], BF16)
            st = [sb("p1_st%d" % j, [128, 8]) for j in range(2)]
            wb = [sb("p1_wb%d" % j, [128, 16, 512], BF16) for j in range(2)]
            stage = sb("p1_stage", [128, NT, 512], BF16)
            qTs = sb("p1_qTs", [128, 4, S], BF16)
            xr = sb("p1_xr", [128, 3 + S])
            acc = sb("p1_acc", [128, S])
            cv = sb("p1_cv", [128, S], BF16)
            sq = sb("p1_sq", [128, 8, 128], BF16)
            qn = sb("p1_qn", [128, 8, 128])
            qb = sb("p1_qb", [128, 8, 128], BF16)
            s4 = sb("p1_s4", [128, 16])
            s4b_tile = sb("p1_s4b", [128, 16])
            rr = [sb("p1_rr%d" % j, [128, 2, 4, 16]) for j in range(4)]
            qkw = sb("p1_qkw", [128, 2, 128])
            cwT = sb("p1_cwT", [128, 24, 4])
            cbT = sb("p1_cbT", [128, 24])
            dtb = sb("p1_dtb", [128, 16])
            dtt = sb("p1_dtt", [128, 16])
            dts = sb("p1_dts", [128, NT, 16])
            tp = [ps("p1_tp%d" % j, [128, 16, 128], BF16) for j in range(2)]
            mm_all = ps("p1_mm", [128, 4, 512])
            mm = [mm_all[:, j, :] for j in range(4)]
            T.dma("sp", qkw, self.qkw.rearrange("a e -> (a e)").partition_broadcast(128), *B("p1_qkw", "qkw"))
            T.dma("sp", cwT, self.cwT, *B("p1_cwT", "cwT"))
            T.dma("sp", cbT, self.cbT, *B("p1_cbT", "cbT"))
            T.dma("sp", dtb, self.dtb.partition_broadcast(128), *B("p1_dtb", "dtb"))
            T.op("pool", lambda e: e.memset(xr[:, 0:3], 0.0), (), (), B("p1_xr"))
            wsrc = self.w_in.rearrange("(c p) n -> p c n", p=128)

            def load_w(n):
                c0, c1 = n * 512, min((n + 1) * 512, DIN)
                T.dma("pool", wb[n % 2][:, :, 0:c1 - c0], wsrc[:, :, c0:c1], *B("p1_wb%d" % (n % 2), "w_in"), partial=False)

            load_w(0)
            def load_x(i):
                r0 = b * S + i * 128
                T.dma("sp", xt[i % 2], self.x[r0:r0 + 128, :], *B("p1_xt%d" % (i % 2), "x"), partial=False)

            load_x(0)

            def stats(i):
                j = i % 2
                if i + 1 < NT:
                    load_x(i + 1)
                for _ in range(4):
                    if self.zq:
                        zo, zi = self.zq.pop()
                        T.dma("sp", zo, zi, *B("xd_d", "zt"))
                X, XN, ST = "p1_xt%d" % j, "p1_xn%d" % j, "p1_st%d" % j
                T.op("dve", lambda e: e.memset(st[j][:, 0:1], 0.0), (), B(ST))
                T.op("act", lambda e: e.activation(out=junk, in_=xt[j], func=AF.Square, accum_out=st[j][:, 0:1]),
                     B(X), B("p1_junk", ST))
                T.op("dve", lambda e: e.tensor_scalar(out=st[j][:, 1:2], in0=st[j][:, 0:1], scalar1=1.0 / D, scalar2=EPS,
                                                      op0=ALU.mult, op1=ALU.add), B(ST), B(ST))
                T.op("act", lambda e: e.activation(out=st[j][:, 2:3], in_=st[j][:, 1:2], func=AF.Sqrt), B(ST), B(ST))
                T.op("dve", lambda e: e.reciprocal(out=st[j][:, 3:4], in_=st[j][:, 2:3]), B(ST), B(ST))
                T.op("act", lambda e: e.activation(out=xn[j], in_=xt[j], func=AF.Copy, scale=st[j][:, 3:4]),
                     B(X, ST), B(XN))

            def tev(i):
                j = i % 2
                XN, TP = "p1_xn%d" % j, "p1_tp%d" % j
                for c in range(16):
                    T.op("pe", lambda e, c=c: e.transpose(out=tp[j][:, c, :], in_=xn[j][:, c * 128:(c + 1) * 128],
                                                          identity=self.ident_b),
                         B(XN, "ident_b"), (), B(TP))
                for c in range(16):
                    o = hT[:, c, i * 128:(i + 1) * 128]
                    T.op("dve", lambda e, c=c, o=o: e.tensor_scalar(out=o, in0=tp[j][:, c, :], scalar1=self.A1T[:, b, c:c + 1],
                                                                    scalar2=self.S1T[:, b, c:c + 1], op0=ALU.mult, op1=ALU.add),
                         B(TP, "A1T", "S1T"), (), B("p1_hT%d" % i))

            stats(0)
            for i in range(NT):
                if i + 1 < NT:
                    stats(i + 1)
                tev(i)
            if "hT" in self.dumps and b == 0:
                for i in range(1, NT):
                    T.wait_bufs("sp", B("p1_hT%d" % i))
                self.dump("hT", hT, T.buf("p1_hT0"), [128, 16, S], BF16)
            tpc = [0]
            nblk = {"p1a": 0, "p1q": 2, "p1v": 6, "p1x": 9}.get(self.stop_after, 15)
            for n in range(nblk):
                if n + 1 < 15:
                    load_w(n + 1)
                self.convert_some(3)
                for _ in range(2):
                    if self.zq:
                        zo, zi = self.zq.pop()
                        T.dma("sp", zo, zi, *B("xd_d", "zt"))
                w = wb[n % 2]
                WN = "p1_wb%d" % (n % 2)
                hh = n % 2
                if n < 4:
                    which = 0 if n < 2 else 1
                    s4b = s4b_tile
                    csets = [
                        dict(sq=sq, qn=qn, qb=qb, rr=rr, s4=s4, SQ="p1_sq", QN="p1_qn", QB="p1_qb",
                             RR=["p1_rr0", "p1_rr1", "p1_rr2", "p1_rr3"], S4="p1_s4"),
                        dict(sq=stage[:, 0:2, :].rearrange("p t (h e) -> p (t h) e", e=128),
                             qn=acc[:, 0:1024].rearrange("p (g e) -> p g e", e=128),
                             qb=stage[:, 2:4, :].rearrange("p t (h e) -> p (t h) e", e=128),
                             rr=[xr[:, 3 + 128 * q_:3 + 128 * (q_ + 1)].rearrange("p (t h e) -> p t h e", t=2, h=4) for q_ in range(4)],
                             s4=s4b, SQ="p1_stage", QN="p1_acc", QB="p1_stage", RR=["p1_xr"] * 4, S4="p1_s4b"),
                    ]
                    def mm_pair(a2):
                        i0 = 2 * a2
                        k0 = i0 % 4
                        for t2 in range(2):
                            i = i0 + t2
                            for kc in range(16):
                                T.op("pe", lambda e, kc=kc, i=i, t2=t2: e.matmul(mm[k0 + t2], lhsT=hT[:, kc, i * 128:(i + 1) * 128], rhs=w[:, kc, :],
                                                                                 start=(kc == 0), stop=(kc == 15)),
                                     B("p1_hT%d" % i, WN), (), B("p1_mm%d" % (k0 + t2)))

                    def chain_pre(a2):
                        cs = csets[a2 % 2]
                        sq_, qn_, qb_, rr_, s4_ = cs["sq"], cs["qn"], cs["qb"], cs["rr"], cs["s4"]
                        SQ, QN, QB, RR, S4 = cs["SQ"], cs["QN"], cs["QB"], cs["RR"], cs["S4"]
                        i0 = 2 * a2
                        k0 = i0 % 4
                        MNs = ["p1_mm%d" % k0, "p1_mm%d" % (k0 + 1)]
                        M8 = mm_all[:, k0:k0 + 2, :].rearrange("p t (h e) -> p (t h) e", e=128)
                        T.op("act", lambda e: e.activation(out=sq_, in_=M8, func=AF.Square), B(*MNs), B(SQ))
                        T.op("dve", lambda e: e.tensor_reduce(out=s4_[:, 0:8], in_=sq_, axis=AX.X, op=ALU.add), B(SQ), B(S4))
                        T.op("dve", lambda e: e.tensor_scalar(out=s4_[:, 0:8], in0=s4_[:, 0:8], scalar1=1.0 / 128, scalar2=EPS,
                                                              op0=ALU.mult, op1=ALU.add), B(S4), B(S4))
                        T.op("act", lambda e: e.activation(out=s4_[:, 0:8], in_=s4_[:, 0:8], func=AF.Sqrt), B(S4), B(S4))
                        T.op("dve", lambda e: e.reciprocal(out=s4_[:, 8:16], in_=s4_[:, 0:8]), B(S4), B(S4))
                        T.op("dve", lambda e: e.tensor_tensor(out=qn_, in0=M8, in1=s4_[:, 8:16].unsqueeze(2).to_broadcast([128, 8, 128]),
                                                              op=ALU.mult), B(S4, *MNs), B(QN))
                        T.op("dve", lambda e: e.tensor_tensor(out=qn_, in0=qn_,
                                                              in1=qkw[:, which, :].unsqueeze(1).to_broadcast([128, 8, 128]),
                                                              op=ALU.mult), B(QN, "p1_qkw"), B(QN))
                        ti = b * NT + i0
                        sh4 = [128, 2, 4, 16]
                        cosb = self.cos_t[:, ti:ti + 2, :].unsqueeze(2).to_broadcast(sh4)
                        sinb = self.sin_t[:, ti:ti + 2, :].unsqueeze(2).to_broadcast(sh4)
                        qn4 = qn_.rearrange("p (t h) e -> p t h e", t=2)
                        t1, t2_ = qn4[:, :, :, 0:16], qn4[:, :, :, 16:32]
                        T.op("dve", lambda e: e.tensor_tensor(out=rr_[0], in0=t1, in1=cosb, op=ALU.mult), B(QN, "cos_t"), (), B(RR[0]))
                        T.op("dve", lambda e: e.tensor_tensor(out=rr_[1], in0=t2_, in1=sinb, op=ALU.mult), B(QN, "sin_t"), (), B(RR[1]))
                        T.op("dve", lambda e: e.tensor_tensor(out=rr_[2], in0=t2_, in1=cosb, op=ALU.mult), B(QN, "cos_t"), (), B(RR[2]))
                        T.op("dve", lambda e: e.tensor_tensor(out=rr_[3], in0=t1, in1=sinb, op=ALU.mult), B(QN, "sin_t"), (), B(RR[3]))
                        T.op("dve", lambda e: e.tensor_tensor(out=t1, in0=rr_[0], in1=rr_[1], op=ALU.subtract),
                             B(RR[0], RR[1]), B(QN))
                        T.op("dve", lambda e: e.tensor_tensor(out=t2_, in0=rr_[2], in1=rr_[3], op=ALU.add),
                             B(RR[2], RR[3]), B(QN))
                        T.op("act", lambda e: e.activation(out=qb_, in_=qn_, func=AF.Copy), B(QN), B(QB))

                    def chain_post(a2):
                        cs = csets[a2 % 2]
                        qb_, QB = cs["qb"], cs["QB"]
                        i0 = 2 * a2
                        tj = tpc[0] % 2
                        tpc[0] += 1
                        TP = "p1_tp%d" % tj
                        for th in range(8):
                            T.op("pe", lambda e, th=th: e.transpose(out=tp[tj][:, th, :], in_=qb_[:, th, :],
                                                                    identity=self.ident_b), B(QB, "ident_b"), (), B(TP))
                        T.op("dve", lambda e: e.tensor_copy(out=qTs[:, :, i0 * 128:(i0 + 2) * 128].rearrange("p h (t e) -> p h t e", t=2),
                                                            in_=tp[tj][:, 0:8, :].rearrange("p (t h) e -> p h t e", t=2)),
                             B(TP), (), B("p1_qTs"))

                    npair = NT // 2
                    mm_pair(0)
                    mm_pair(1)
                    chain_pre(0)
                    for a2 in range(npair):
                        if a2 + 1 < npair:
                            chain_pre(a2 + 1)
                        if a2 + 2 < npair:
                            mm_pair(a2 + 2)
                        chain_post(a2)
                    dst = self.qT_d if which == 0 else self.kT_d
                    dn = "qT_d" if which == 0 else "kT_d"
                    T.dma("sp", dst[b, hh * 4:(hh + 1) * 4].rearrange("h e t -> e h t"), qTs, *B(dn, "p1_qTs"))
                elif n < 8:
                    for i in range(NT):
                        M = mm[i % 4]
                        MN = "p1_mm%d" % (i % 4)
                        for kc in range(16):
                            T.op("pe", lambda e, kc=kc: e.matmul(M, lhsT=hT[:, kc, i * 128:(i + 1) * 128], rhs=w[:, kc, :],
                                                                 start=(kc == 0), stop=(kc == 15)),
                                 B("p1_hT%d" % i, WN), (), B(MN))
                        if i % 2 == 0:
                            T.op("act", lambda e: e.activation(out=stage[:, i, :], in_=M, func=AF.Copy), B(MN), (), B("p1_stage"))
                        else:
                            T.op("dve", lambda e: e.tensor_copy(out=stage[:, i, :], in_=M), B(MN), (), B("p1_stage"))
                    dst, dn = (self.v_d, "v_d") if n < 6 else (self.z_d, "z_d")
                    T.dma("sp", dst[b].rearrange("(i p) c -> p i c", p=128)[:, :, hh * 512:(hh + 1) * 512], stage,
                          *B(dn, "p1_stage"))
                elif n < 14:
                    cvs = [(cv, "p1_cv"), (junk, "p1_junk")]

                    def st1(m):
                        ch = (n - 8) * 4 + m
                        cvt, CVN = cvs[m % 2]
                        for tg in range(4):
                            M = mm[tg]
                            MN = "p1_mm%d" % tg
                            for kc in range(16):
                                T.op("pe", lambda e, kc=kc, M=M, tg=tg: e.matmul(
                                    M, lhsT=w[:, kc, m * 128:(m + 1) * 128], rhs=hT[:, kc, tg * 512:(tg + 1) * 512],
                                    start=(kc == 0), stop=(kc == 15)),
                                     B(WN, *["p1_hT%d" % (tg * 4 + q) for q in range(4)]), (), B(MN))
                            T.op("act", lambda e, M=M, tg=tg: e.activation(out=xr[:, 3 + tg * 512:3 + (tg + 1) * 512], in_=M, func=AF.Copy),
                                 B(MN), (), B("p1_xr"))
                        T.op("dve", lambda e: e.tensor_scalar(out=acc, in0=xr[:, 0:S], scalar1=cwT[:, ch, 0:1], scalar2=None,
                                                              op0=ALU.mult), B("p1_xr", "p1_cwT"), B("p1_acc"))
                        for jj in range(1, 4):
                            T.op("dve", lambda e, jj=jj: e.scalar_tensor_tensor(out=acc, in0=xr[:, jj:jj + S], scalar=cwT[:, ch, jj:jj + 1],
                                                                                in1=acc, op0=ALU.mult, op1=ALU.add),
                                 B("p1_xr", "p1_cwT", "p1_acc"), B("p1_acc"))
                        T.op("act", lambda e: e.activation(out=cvt, in_=acc, func=AF.Silu, bias=cbT[:, ch:ch + 1]),
                             B("p1_acc", "p1_cbT"), B(CVN))

                    def st2(m):
                        cvt, CVN = cvs[m % 2]
                        if n < 12:
                            tj = tpc[0] % 2
                            tpc[0] += 1
                            TP = "p1_tp%d" % tj
                            for i in range(NT):
                                T.op("pe", lambda e, i=i: e.transpose(out=tp[tj][:, i, :], in_=cvt[:, i * 128:(i + 1) * 128],
                                                                      identity=self.ident_b), B(CVN, "ident_b"), (), B(TP))
                            T.op("dve", lambda e: e.tensor_copy(out=stage[:, :, m * 128:(m + 1) * 128], in_=tp[tj]),
                                 B(TP), (), B("p1_stage"))
                        if n >= 10:
                            g = ((n - 10) % 2) * 4 + m
                            dst, dn = (self.BT_d, "BT_d") if n < 12 else (self.CT_d, "CT_d")
                            T.dma("sp", dst[b, g], cvt, *B("%s_%d" % (dn, m % 2), CVN))

                    st1(0)
                    for m in range(4):
                        if m + 1 < 4:
                            st1(m + 1)
                        st2(m)
                    if n < 12:
                        dst, dn = (self.xs_d, "xs_d") if n < 10 else (self.bt_d, "bt_d")
                        T.dma("sp", dst[b].rearrange("(i p) c -> p i c", p=128)[:, :, hh * 512:(hh + 1) * 512], stage,
                              *B(dn, "p1_stage"))
                else:
                    M = mm[0]
                    for i in range(NT):
                        for kc in range(16):
                            T.op("pe", lambda e, kc=kc, i=i: e.matmul(M[:, i * 16:(i + 1) * 16], lhsT=hT[:, kc, i * 128:(i + 1) * 128], rhs=w[:, kc, 0:16],
                                                                      start=(kc == 0), stop=(kc == 15)),
                                 B("p1_hT%d" % i, WN), (), B("p1_mm0"))
                    dflat = dts.rearrange("p i h -> p (i h)")
                    T.op("dve", lambda e: e.tensor_tensor(out=dts, in0=M[:, 0:NT * 16].rearrange("p (i h) -> p i h", h=16),
                                                          in1=dtb.unsqueeze(1).to_broadcast([128, NT, 16]), op=ALU.add),
                         B("p1_mm0", "p1_dtb"), B("p1_dts"))
                    T.op("act", lambda e: e.activation(out=dflat, in_=dflat, func=AF.Exp), B("p1_dts"), B("p1_dts"))
                    T.op("act", lambda e: e.activation(out=dflat, in_=dflat, func=AF.Ln, bias=1.0), B("p1_dts"), B("p1_dts"))
                    T.dma("sp", self.dt_d[b].rearrange("(i p) h -> p i h", p=128), dts, *B("dt_d", "p1_dts"))
            T.barrier()

    def p2(self):
        nc, T, B = self.nc, self.T, self.B
        with contextlib.ExitStack() as es:
            sb, ps = (lambda *a, **k: self.sb(es, *a, **k)), (lambda *a, **k: self.ps(es, *a, **k))
            tbx = sb("p2_tbx", [128, 19 * 128], BF16)
            ones = sb("p2_ones", [128, 128], BF16)
            V = sb("p2_V", [128, NT, 1024], BF16)
            qT = [sb("p2_qT%d" % j, [128, S], BF16) for j in range(2)]
            kT = [sb("p2_kT%d" % j, [128, S], BF16) for j in range(2)]
            P = [sb("p2_P%d" % j, [128, 512], BF16) for j in range(4)]
            rden = sb("p2_rden", [128, 512])
            yT = [sb("p2_yT%d" % j, [128, S], BF16) for j in range(2)]
            S_ps = [ps("p2_S%d" % j, [128, 512]) for j in range(4)]
            O_ps = [ps("p2_O%d" % j, [128, 512]) for j in range(2)]
            D_ps = [ps("p2_D%d" % j, [128, 512]) for j in range(2)]
            T.dma("pool", tbx, self.c_tbx, *B("p2_tbx", "c_tbx"))
            T.op("dve", lambda e: e.memset(ones, 1.0), (), B("p2_ones"))
            scale = 1.0 / math.sqrt(128.0)
            it = 0
            cc = 0
            pj = 0

            def load_qk(b, h, j):
                T.dma("sp", qT[j], self.qT_d[b, h], *B("p2_qT%d" % j, "qT_d"), partial=False)
                T.dma("sp", kT[j], self.kT_d[b, h], *B("p2_kT%d" % j, "kT_d"), partial=False)

            for b in range(NB):
                T.dma("sp", V, self.v_d[b].rearrange("(i p) c -> p i c", p=128), *B("p2_V", "v_d"), partial=False)
                load_qk(b, 0, it % 2)
                for h in range(8):
                    j = it % 2
                    it += 1
                    if h + 1 < 8:
                        load_qk(b, h + 1, it % 2)
                    QN, KN, YN = "p2_qT%d" % j, "p2_kT%d" % j, "p2_yT%d" % j
                    units = [(c, kb) for c in range(4) for kb in range(4 * c + 4)]
                    sbuf_of = {}

                    def emit_S(u):
                        c, kb = units[u]
                        sj = (pj0 + u) % 4
                        sbuf_of[u] = sj
                        SN = "p2_S%d" % sj
                        T.op("pe", lambda e: e.matmul(S_ps[sj], lhsT=kT[j][:, kb * 128:(kb + 1) * 128],
                                                      rhs=qT[j][:, c * 512:(c + 1) * 512], start=True, stop=False),
                             B(KN, QN), B(SN))
                        i0 = (4 * c - kb + 3) * 128
                        T.op("pe", lambda e: e.matmul(S_ps[sj], lhsT=self.ident_b, rhs=tbx[:, i0:i0 + 512],
                                                      start=False, stop=True), B("ident_b", "p2_tbx"), (), B(SN))

                    pj0 = pj
                    LOOK = 2
                    for u in range(min(LOOK, len(units))):
                        emit_S(u)
                    for u, (c, kb) in enumerate(units):
                        if u + LOOK < len(units):
                            emit_S(u + LOOK)
                        if kb == 0:
                            oj = cc % 2
                            cc += 1
                        ON, DN = "p2_O%d" % oj, "p2_D%d" % oj
                        nkb = 4 * c + 4
                        sj = sbuf_of[u]
                        SN, PN = "p2_S%d" % sj, "p2_P%d" % sj
                        T.op("act", lambda e: e.activation(out=P[sj], in_=S_ps[sj], func=AF.Exp, scale=scale),
                             B(SN), B(PN))
                        T.op("pe", lambda e: e.matmul(O_ps[oj], lhsT=V[:, kb, h * 128:(h + 1) * 128], rhs=P[sj],
                                                      start=(kb == 0), stop=(kb == nkb - 1)),
                             B("p2_V", PN), (), B(ON))
                        T.op("pe", lambda e: e.matmul(D_ps[oj], lhsT=ones, rhs=P[sj],
                                                      start=(kb == 0), stop=(kb == nkb - 1)),
                             B("p2_ones", PN), (), B(DN))
                        if kb == nkb - 1:
                            T.op("dve", lambda e: e.reciprocal(out=rden, in_=D_ps[oj]), B(DN), B("p2_rden"))
                            T.op("dve", lambda e: e.tensor_tensor(out=yT[j][:, c * 512:(c + 1) * 512], in0=O_ps[oj], in1=rden,
                                                                  op=ALU.mult), B(ON, "p2_rden"), (), B(YN))
                    pj += len(units)
                    T.dma("sp", self.yT_d[b, h], yT[j], *B("yT_d", YN))
            T.barrier()

    def p3(self):
        nc, T, B = self.nc, self.T, self.B
        with contextlib.ExitStack() as es:
            sb, ps = (lambda *a, **k: self.sb(es, *a, **k)), (lambda *a, **k: self.ps(es, *a, **k))
            U = sb("p3_U", [128, 128])
            onesf = sb("p3_onesf", [128, 128])
            SBm = sb("p3_SBm", [128, 4, 128], BF16)
            a_bc = sb("p3_a", [128, 16])
            dsk = sb("p3_dsk", [128, 16])
            nw = sb("p3_nw", [128, 1024])
            state = sb("p3_state", [128, 16, 64])
            state_b = sb("p3_stateb", [128, 16, 64], BF16)
            yTs = sb("p3_yTs", [128, 8, S], BF16)
            xs = [sb("p3_xs%d" % j, [128, 16, 64], BF16) for j in range(2)]
            bt = [sb("p3_bt%d" % j, [128, 1024], BF16) for j in range(2)]
            BT = [sb("p3_BT%d" % j, [128, 8, 128], BF16) for j in range(2)]
            CT = [sb("p3_CT%d" % j, [128, 8, 128], BF16) for j in range(2)]
            zc = [sb("p3_z%d" % j, [128, 1024], BF16) for j in range(2)]
            dtc = [sb("p3_dt%d" % j, [128, 16]) for j in range(2)]
            dta = sb("p3_dta", [128, 16])
            acs = sb("p3_acs", [128, 16])
            nacs = sb("p3_nacs", [128, 16])
            eacs = sb("p3_eacs", [128, 16])
            last = sb("p3_last", [128, 16])
            dte = sb("p3_dte", [128, 16])
            cd = sb("p3_cd", [128, 16])
            rhsbig = sb("p3_rhsbig", [128, 16, 128])
            dec = sb("p3_dec", [128, 8, 128])
            GT = sb("p3_GT", [128, 8, 128], BF16)
            xdt = sb("p3_xdt", [128, 16, 64], BF16)
            xdtd = sb("p3_xdtd", [128, 16, 64], BF16)
            y1 = sb("p3_y1", [128, 16, 64])
            tmp = sb("p3_tmp", [128, 16, 64])
            sz = sb("p3_sz", [128, 1024])
            s8 = sb("p3_s8", [128, 16])
            y4 = sb("p3_y4", [128, 1024], BF16)
            acs_ps = ps("p3_acsps", [128, 16])
            acsb = ps("p3_acsb", [128, 8, 128])
            cb = ps("p3_cb", [128, 4, 128])
            y_ps = ps("p3_yps", [128, 512])
            yo_ps = ps("p3_yops", [128, 512])
            Sc_ps = ps("p3_Scps", [128, 512])
            tp = ps("p3_tp", [128, 8, 128], BF16)
            T.dma("sp", U, self.c_U, *B("p3_U", "c_U"))
            for q in range(4):
                T.dma("pool", SBm[:, q, :], self.c_SB, *B("p3_SBm", "c_SB"))
            T.op("dve", lambda e: e.memset(onesf, 1.0), (), B("p3_onesf"))
            T.dma("sp", a_bc, self.alog.partition_broadcast(128), *B("p3_a", "alog"))
            T.dma("sp", dsk, self.dskip.partition_broadcast(128), *B("p3_dsk", "dskip"))
            T.dma("sp", nw, self.ssm_nw.partition_broadcast(128), *B("p3_nw", "ssm_nw"))
            T.op("act", lambda e: e.activation(out=a_bc, in_=a_bc, func=AF.Exp), B("p3_a"), B("p3_a"))
            T.op("dve", lambda e: e.tensor_scalar(out=a_bc, in0=a_bc, scalar1=-1.0, scalar2=None, op0=ALU.mult), B("p3_a"), B("p3_a"))

            def load(b, i, j):
                r0 = i * 128
                T.dma("sp", xs[j].rearrange("p h e -> p (h e)"), self.xs_d[b, r0:r0 + 128, :], *B("p3_xs%d" % j, "xs_d"), partial=False)
                T.dma("sp", bt[j], self.bt_d[b, r0:r0 + 128, :], *B("p3_bt%d" % j, "bt_d"), partial=False)
                T.dma("sp", BT[j], self.BT_d[b, :, :, r0:r0 + 128].rearrange("g n t -> n g t"), *B("p3_BT%d" % j, "BT_d"), partial=False)
                T.dma("sp", CT[j], self.CT_d[b, :, :, r0:r0 + 128].rearrange("g n t -> n g t"), *B("p3_CT%d" % j, "CT_d"), partial=False)
                T.dma("sp", zc[j], self.z_d[b, r0:r0 + 128, :], *B("p3_z%d" % j, "z_d"), partial=False)
                T.dma("sp", dtc[j], self.dt_d[b, r0:r0 + 128, :], *B("p3_dt%d" % j, "dt_d"), partial=False)

            it = 0
            for b in range(NB):
                T.op("dve", lambda e: e.memset(state, 0.0), (), B("p3_state"))
                T.op("dve", lambda e: e.memset(state_b, 0.0), (), B("p3_stateb"))
                load(b, 0, it % 2)
                for i in range(NT):
                    j = it % 2
                    it += 1
                    if i + 1 < NT:
                        load(b, i + 1, it % 2)
                    T.wait_bufs("pool", B("p3_dta"))
                    self.convert_some(2)
                    XS, BTN, BTT, CTT, ZN, DTN = ("p3_xs%d" % j, "p3_bt%d" % j, "p3_BT%d" % j, "p3_CT%d" % j,
                                                  "p3_z%d" % j, "p3_dt%d" % j)
                    sh = [128, 16, 64]
                    T.op("dve", lambda e: e.tensor_tensor(out=dta, in0=dtc[j], in1=a_bc, op=ALU.mult), B(DTN, "p3_a"), B("p3_dta"))
                    T.op("pe", lambda e: e.matmul(acs_ps, lhsT=U, rhs=dta, start=True, stop=True), B("p3_U", "p3_dta"), B("p3_acsps"))
                    T.op("dve", lambda e: e.tensor_copy(out=acs, in_=acs_ps), B("p3_acsps"), B("p3_acs"))
                    T.op("dve", lambda e: e.tensor_scalar(out=nacs, in0=acs_ps, scalar1=-1.0, scalar2=None, op0=ALU.mult),
                         B("p3_acsps"), B("p3_nacs"))
                    T.op("act", lambda e: e.activation(out=eacs, in_=acs, func=AF.Exp), B("p3_acs"), B("p3_eacs"))
                    T.op("dve", lambda e: e.tensor_tensor(out=rhsbig, in0=U.unsqueeze(1).to_broadcast([128, 16, 128]),
                                                          in1=dta.unsqueeze(2).to_broadcast([128, 16, 128]), op=ALU.mult),
                         B("p3_U", "p3_dta"), B("p3_rhsbig"))
                    T.op("dve", lambda e: e.tensor_tensor(out=xdt, in0=xs[j], in1=dtc[j].unsqueeze(2).to_broadcast(sh), op=ALU.mult),
                         B(XS, DTN), B("p3_xdt"))
                    for hh in range(2):
                        h0 = hh * 8
                        for q in range(2):
                            T.op("pe", lambda e: e.matmul(acsb[:, q * 4:(q + 1) * 4, :].rearrange("p h l -> p (h l)"), lhsT=onesf,
                                                          rhs=rhsbig[:, h0 + q * 4:h0 + (q + 1) * 4, :].rearrange("p h l -> p (h l)"),
                                                          start=True, stop=False), B("p3_onesf", "p3_rhsbig"), (), B("p3_acsb"))
                            T.op("pe", lambda e: e.matmul(acsb[:, q * 4:(q + 1) * 4, :].rearrange("p h l -> p (h l)"), lhsT=self.ident_b,
                                                          rhs=SBm.rearrange("p h l -> p (h l)"), start=False, stop=True),
                                 B("ident_b", "p3_SBm"), (), B("p3_acsb"))
                        T.op("dve", lambda e: e.tensor_copy(out=last[:, h0:h0 + 8], in_=acsb[:, :, 127]), B("p3_acsb"), (), B("p3_last"))
                        for hl in range(8):
                            h = h0 + hl
                            T.op("act", lambda e, h=h, hl=hl: e.activation(out=dec[:, hl, :], in_=acsb[:, hl, :], func=AF.Exp,
                                                                           bias=nacs[:, h:h + 1]),
                                 B("p3_acsb", "p3_nacs"), (), B("p3_dec"))
                        for gl in range(4):
                            g = hh * 4 + gl
                            T.op("pe", lambda e, g=g, gl=gl: e.matmul(cb[:, gl, :], lhsT=BT[j][:, g, :], rhs=CT[j][:, g, :],
                                                                      start=True, stop=True), B(BTT, CTT), (), B("p3_cb"))
                        T.op("dve", lambda e: e.tensor_tensor(out=GT.rearrange("p (g r) l -> p g r l", r=2),
                                                              in0=cb.unsqueeze(2).to_broadcast([128, 4, 2, 128]),
                                                              in1=dec.rearrange("p (g r) l -> p g r l", r=2), op=ALU.mult),
                             B("p3_cb", "p3_dec"), B("p3_GT"))
                        for hl in range(8):
                            h = h0 + hl
                            T.op("pe", lambda e, h=h, hl=hl: e.matmul(y_ps[:, hl * 64:(hl + 1) * 64], lhsT=GT[:, hl, :], rhs=xdt[:, h, :],
                                                                      start=True, stop=True), B("p3_GT", "p3_xdt"), (), B("p3_yps"))
                        for gl in range(4):
                            g = hh * 4 + gl
                            T.op("pe", lambda e, g=g, gl=gl: e.matmul(yo_ps[:, gl * 128:(gl + 1) * 128], lhsT=CT[j][:, g, :],
                                                                      rhs=state_b[:, 2 * g:2 * g + 2, :].rearrange("p h e -> p (h e)"),
                                                                      start=True, stop=True), B(CTT, "p3_stateb"), (), B("p3_yops"))
                        T.op("dve", lambda e: e.tensor_tensor(out=y1[:, h0:h0 + 8, :], in0=yo_ps.rearrange("p (h e) -> p h e", e=64),
                                                              in1=eacs[:, h0:h0 + 8].unsqueeze(2).to_broadcast([128, 8, 64]), op=ALU.mult),
                             B("p3_yops", "p3_eacs"), (), B("p3_y1"))
                        T.op("dve", lambda e: e.tensor_tensor(out=y1[:, h0:h0 + 8, :], in0=y1[:, h0:h0 + 8, :],
                                                              in1=y_ps.rearrange("p (h e) -> p h e", e=64), op=ALU.add),
                             B("p3_yps", "p3_y1"), (), B("p3_y1"))
                        T.op("dve", lambda e: e.tensor_tensor(out=dte[:, h0:h0 + 8], in0=last[:, h0:h0 + 8], in1=acs[:, h0:h0 + 8],
                                                              op=ALU.subtract), B("p3_last", "p3_acs"), (), B("p3_dte"))
                        T.op("act", lambda e: e.activation(out=dte[:, h0:h0 + 8], in_=dte[:, h0:h0 + 8], func=AF.Exp),
                             B("p3_dte"), (), B("p3_dte"))
                        T.op("act", lambda e: e.activation(out=cd[:, h0:h0 + 8], in_=last[:, h0:h0 + 8], func=AF.Exp),
                             B("p3_last"), (), B("p3_cd"))
                        T.op("dve", lambda e: e.tensor_tensor(out=xdtd[:, h0:h0 + 8, :], in0=xdt[:, h0:h0 + 8, :],
                                                              in1=dte[:, h0:h0 + 8].unsqueeze(2).to_broadcast([128, 8, 64]), op=ALU.mult),
                             B("p3_xdt", "p3_dte"), (), B("p3_xdtd"))
                        for gl in range(4):
                            g = hh * 4 + gl
                            T.op("pe", lambda e, g=g, gl=gl: e.matmul(Sc_ps[:, gl * 128:(gl + 1) * 128], lhsT=bt[j][:, g * 128:(g + 1) * 128],
                                                                      rhs=xdtd[:, 2 * g:2 * g + 2, :].rearrange("p h e -> p (h e)"),
                                                                      start=True, stop=True), B(BTN, "p3_xdtd"), (), B("p3_Scps"))
                        T.op("dve", lambda e: e.tensor_tensor(out=state[:, h0:h0 + 8, :], in0=state[:, h0:h0 + 8, :],
                                                              in1=cd[:, h0:h0 + 8].unsqueeze(2).to_broadcast([128, 8, 64]), op=ALU.mult),
                             B("p3_cd", "p3_state"), (), B("p3_state"))
                        T.op("dve", lambda e: e.tensor_tensor(out=state[:, h0:h0 + 8, :], in0=state[:, h0:h0 + 8, :],
                                                              in1=Sc_ps.rearrange("p (h e) -> p h e", e=64), op=ALU.add),
                             B("p3_Scps", "p3_state"), (), B("p3_state"))
                        T.op("act", lambda e: e.activation(out=state_b[:, h0:h0 + 8, :], in_=state[:, h0:h0 + 8, :], func=AF.Copy),
                             B("p3_state"), (), B("p3_stateb"))
                    T.op("dve", lambda e: e.tensor_tensor(out=tmp, in0=xs[j], in1=dsk.unsqueeze(2).to_broadcast(sh), op=ALU.mult),
                         B(XS, "p3_dsk"), B("p3_tmp"))
                    T.op("dve", lambda e: e.tensor_tensor(out=y1, in0=y1, in1=tmp, op=ALU.add), B("p3_y1", "p3_tmp"), B("p3_y1"))
                    T.op("act", lambda e: e.activation(out=sz, in_=zc[j], func=AF.Silu), B(ZN), B("p3_sz"))
                    y1f = y1.rearrange("p h e -> p (h e)")
                    T.op("dve", lambda e: e.tensor_tensor(out=y1f, in0=y1f, in1=sz, op=ALU.mult), B("p3_y1", "p3_sz"), B("p3_y1"))
                    T.op("dve", lambda e: e.tensor_tensor(out=sz, in0=y1f, in1=y1f, op=ALU.mult), B("p3_y1"), B("p3_sz"))
                    T.op("dve", lambda e: e.tensor_reduce(out=s8[:, 0:8], in_=sz.rearrange("p (g c) -> p g c", c=128), axis=AX.X, op=ALU.add),
                         B("p3_sz"), B("p3_s8"))
                    T.op("dve", lambda e: e.tensor_scalar(out=s8[:, 0:8], in0=s8[:, 0:8], scalar1=1.0 / 128, scalar2=EPS,
                                                          op0=ALU.mult, op1=ALU.add), B("p3_s8"), B("p3_s8"))
                    T.op("act", lambda e: e.activation(out=s8[:, 0:8], in_=s8[:, 0:8], func=AF.Sqrt), B("p3_s8"), B("p3_s8"))
                    T.op("dve", lambda e: e.reciprocal(out=s8[:, 8:16], in_=s8[:, 0:8]), B("p3_s8"), B("p3_s8"))
                    T.op("dve", lambda e: e.tensor_tensor(out=sz.rearrange("p (g c) -> p g c", c=128), in0=y1.rearrange("p (g r) e -> p g (r e)", r=2),
                                                          in1=s8[:, 8:16].unsqueeze(2).to_broadcast([128, 8, 128]), op=ALU.mult),
                         B("p3_y1", "p3_s8"), B("p3_sz"))
                    T.op("dve", lambda e: e.tensor_tensor(out=y4, in0=sz, in1=nw, op=ALU.mult), B("p3_sz", "p3_nw"), B("p3_y4"))
                    for g in range(8):
                        T.op("pe", lambda e, g=g: e.transpose(out=tp[:, g, :], in_=y4[:, g * 128:(g + 1) * 128], identity=self.ident_b),
                             B("p3_y4", "ident_b"), (), B("p3_tp"))
                    T.op("dve", lambda e: e.tensor_copy(out=yTs[:, :, i * 128:(i + 1) * 128], in_=tp), B("p3_tp"), (), B("p3_yTs"))
                T.dma("sp", self.yT_d[b, 8:16].rearrange("g c t -> c g t"), yTs, *B("yT_d", "p3_yTs"))
            T.barrier()

    def p4(self):
        nc, T, B = self.nc, self.T, self.B
        while self.zq:
            zo, zi = self.zq.pop()
            T.dma("sp", zo, zi, *B("xd_d", "zt"))
        with contextlib.ExitStack() as es:
            sb, ps = (lambda *a, **k: self.sb(es, *a, **k)), (lambda *a, **k: self.ps(es, *a, **k))
            wo = sb("p4_wo", [128, 16, D], BF16)
            wr = sb("p4_wr", [128, 16, 72])
            br = sb("p4_br", [128, 72])
            Ls = sb("p4_Ls", [128, 128], BF16)
            Lf = sb("p4_Lf", [128, 128])
            onesb = sb("p4_onesb", [128, 128], BF16)
            g1 = sb("p4_g1", [128, D])
            A2 = sb("p4_A2", [128, D])
            sh2 = sb("p4_sh2", [128, D])
            yTg = [sb("p4_yTg%d" % j, [128, 16, 512], BF16) for j in range(2)]
            xt = [sb("p4_xt%d" % j, [128, D]) for j in range(2)]
            x1 = [sb("p4_x1%d" % j, [128, D]) for j in range(2)]
            h2 = sb("p4_h2", [128, D])
            h2b = [sb("p4_h2b%d" % j, [128, D], BF16) for j in range(2)]
            junk = sb("p4_junk", [128, D], BF16)
            h2T = sb("p4_h2T", [128, 16, 128])
            st = sb("p4_st", [128, 8])
            lg = sb("p4_lg", [128, 72])
            sm = sb("p4_sm", [128, 16])
            ohg = sb("p4_ohg", [128, 8])
            ge = sb("p4_ge", [128, 8])
            t64 = sb("p4_t64", [128, 8, 8])
            esel = sb("p4_esel", [128, 8])
            esel2 = sb("p4_esel2", [128, 8])
            oh1 = sb("p4_oh1", [128, 8])
            oh2 = sb("p4_oh2", [128, 8])
            OH1 = sb("p4_OH1", [128, 8, 8])
            OH2 = sb("p4_OH2", [128, 8, 8])
            Mf = sb("p4_Mf", [128, NE])
            Mb = sb("p4_Mb", [128, NE], BF16)
            base = sb("p4_base", [128, NE])
            rk = sb("p4_rk", [128, NE])
            dsf = sb("p4_dsf", [128, 8])
            mm = [ps("p4_mm%d" % j, [128, 512]) for j in range(4)]
            tpf = [ps("p4_tpf%d" % j, [128, 4, 128]) for j in range(2)]
            lg_ps = ps("p4_lgps", [128, 72])
            cum_ps = ps("p4_cumps", [128, 2, NE])
            T.dma("pool", wo, self.w_out.rearrange("(c p) n -> p c n", p=128), *B("p4_wo", "w_out"))
            T.dma("sp", wr, self.wr.rearrange("(c p) n -> p c n", p=128), *B("p4_wr", "wr"))
            T.dma("sp", br, self.br.partition_broadcast(128), *B("p4_br", "br"))
            T.dma("sp", Lf, self.c_U, *B("p4_Lf", "c_U"))
            T.op("dve", lambda e: e.tensor_tensor(out=Ls, in0=Lf, in1=self.ident_f, op=ALU.subtract), B("p4_Lf", "ident_f"), B("p4_Ls"))
            T.op("dve", lambda e: e.memset(onesb, 1.0), (), B("p4_onesb"))
            T.op("dve", lambda e: e.memset(self.runtot, 0.0), (), B("runtot"))
            it = 0
            for b in range(NB):
                md = self.mod_d[b]
                T.dma("sp", g1, md[2 * D:3 * D].partition_broadcast(128), *B("p4_g1", "mod_d"), partial=False)
                T.dma("sp", sh2, md[3 * D:4 * D].partition_broadcast(128), *B("p4_sh2", "mod_d"), partial=False)
                T.dma("sp", A2, md[4 * D:5 * D].partition_broadcast(128), *B("p4_A2", "mod_d"), partial=False)
                T.dma("sp", h2, self.n2w.partition_broadcast(128), *B("p4_h2", "n2w"), partial=False)
                T.op("dve", lambda e: e.scalar_tensor_tensor(out=A2, in0=A2, scalar=1.0, in1=h2, op0=ALU.add, op1=ALU.mult),
                     B("p4_A2", "p4_h2"), B("p4_A2"))

                def load_y(tg4, j):
                    T.dma("sp", yTg[j], self.yT_d[b, :, :, tg4 * 512:(tg4 + 1) * 512].rearrange("k c t -> c k t"),
                          *B("p4_yTg%d" % j, "yT_d"), partial=False)

                def load_x(i, j):
                    r0 = b * S + i * 128
                    T.dma("sp", xt[j], self.x[r0:r0 + 128, :], *B("p4_xt%d" % j, "x"), partial=False)

                load_y(0, 0)
                load_x(0, it % 2)
                for i in range(NT):
                    j = it % 2
                    it += 1
                    ti = b * NT + i
                    tg4, tt = i // 4, i % 4
                    yj = tg4 % 2
                    if tt == 0 and tg4 + 1 < 4:
                        load_y(tg4 + 1, (tg4 + 1) % 2)
                    if i + 1 < NT:
                        load_x(i + 1, it % 2)
                    T.wait_bufs("pool", B("p4_st"))
                    self.convert_some(2)
                    YN, XN, X1N, HBN = "p4_yTg%d" % yj, "p4_xt%d" % j, "p4_x1%d" % j, "p4_h2b%d" % j
                    for nq in range(4):
                        for kc in range(16):
                            T.op("pe", lambda e, nq=nq, kc=kc: e.matmul(mm[nq], lhsT=yTg[yj][:, kc, tt * 128:(tt + 1) * 128],
                                                                        rhs=wo[:, kc, nq * 512:(nq + 1) * 512],
                                                                        start=(kc == 0), stop=(kc == 15)),
                                 B(YN, "p4_wo"), (), B("p4_mm%d" % nq))
                    for nq in range(4):
                        sl = slice(nq * 512, (nq + 1) * 512)
                        T.op("dve", lambda e, nq=nq, sl=sl: e.tensor_tensor(out=x1[j][:, sl], in0=mm[nq], in1=g1[:, sl], op=ALU.mult),
                             B("p4_mm%d" % nq, "p4_g1"), (), B(X1N))
                        T.op("dve", lambda e, sl=sl: e.tensor_tensor(out=x1[j][:, sl], in0=x1[j][:, sl], in1=xt[j][:, sl], op=ALU.add),
                             B(XN, X1N), (), B(X1N))
                    r0 = b * S + i * 128
                    T.dma("sp", self.x1_d[r0:r0 + 128, :], x1[j], *B("x1_d", X1N))
                    T.op("dve", lambda e: e.memset(st[:, 0:1], 0.0), (), B("p4_st"))
                    T.op("act", lambda e: e.activation(out=junk, in_=x1[j], func=AF.Square, accum_out=st[:, 0:1]),
                         B(X1N), B("p4_junk", "p4_st"))
                    T.op("dve", lambda e: e.tensor_scalar(out=st[:, 1:2], in0=st[:, 0:1], scalar1=1.0 / D, scalar2=EPS,
                                                          op0=ALU.mult, op1=ALU.add), B("p4_st"), B("p4_st"))
                    T.op("act", lambda e: e.activation(out=st[:, 2:3], in_=st[:, 1:2], func=AF.Sqrt), B("p4_st"), B("p4_st"))
                    T.op("dve", lambda e: e.reciprocal(out=st[:, 3:4], in_=st[:, 2:3]), B("p4_st"), B("p4_st"))
                    T.op("act", lambda e: e.activation(out=h2, in_=x1[j], func=AF.Copy, scale=st[:, 3:4]), B(X1N, "p4_st"), B("p4_h2"))
                    T.op("dve", lambda e: e.tensor_tensor(out=h2, in0=h2, in1=A2, op=ALU.mult), B("p4_h2", "p4_A2"), B("p4_h2"))
                    T.op("dve", lambda e: e.tensor_tensor(out=h2, in0=h2, in1=sh2, op=ALU.add), B("p4_h2", "p4_sh2"), B("p4_h2"))
                    T.op("act", lambda e: e.activation(out=h2b[j], in_=h2, func=AF.Copy), B("p4_h2"), B(HBN))
                    for q in range(4):
                        tj = q % 2
                        for c4 in range(4):
                            c = q * 4 + c4
                            T.op("pe", lambda e, c=c, c4=c4, tj=tj: e.transpose(out=tpf[tj][:, c4, :], in_=h2[:, c * 128:(c + 1) * 128],
                                                                                identity=self.ident_f),
                                 B("p4_h2", "ident_f"), (), B("p4_tpf%d" % tj))
                        if q % 2 == 0:
                            T.op("dve", lambda e, q=q, tj=tj: e.tensor_copy(out=h2T[:, q * 4:(q + 1) * 4, :], in_=tpf[tj]),
                                 B("p4_tpf%d" % tj), (), B("p4_h2T"))
                        else:
                            T.op("act", lambda e, q=q, tj=tj: e.activation(out=h2T[:, q * 4:(q + 1) * 4, :], in_=tpf[tj], func=AF.Copy),
                                 B("p4_tpf%d" % tj), (), B("p4_h2T"))
                    for kc in range(16):
                        T.op("pe", lambda e, kc=kc: e.matmul(lg_ps, lhsT=h2T[:, kc, :], rhs=wr[:, kc, :], start=(kc == 0), stop=(kc == 15)),
                             B("p4_h2T", "p4_wr"), (), B("p4_lgps"))
                    T.op("dve", lambda e: e.tensor_tensor(out=lg, in0=lg_ps, in1=br, op=ALU.add), B("p4_lgps", "p4_br"), B("p4_lg"))
                    SM = B("p4_sm")
                    T.op("dve", lambda e: e.tensor_reduce(out=sm[:, 0:1], in_=lg[:, 0:8], axis=AX.X, op=ALU.max), B("p4_lg"), SM)
                    T.op("dve", lambda e: e.tensor_scalar(out=ohg, in0=lg[:, 0:8], scalar1=sm[:, 0:1], scalar2=None, op0=ALU.is_equal),
                         B("p4_lg", "p4_sm"), B("p4_ohg"))
                    T.op("dve", lambda e: e.tensor_scalar(out=ge, in0=lg[:, 0:8], scalar1=sm[:, 0:1], scalar2=None, op0=ALU.subtract),
                         B("p4_lg", "p4_sm"), B("p4_ge"))
                    T.op("act", lambda e: e.activation(out=ge, in_=ge, func=AF.Exp), B("p4_ge"), B("p4_ge"))
                    T.op("dve", lambda e: e.tensor_reduce(out=sm[:, 1:2], in_=ge, axis=AX.X, op=ALU.add), B("p4_ge"), SM)
                    T.op("dve", lambda e: e.reciprocal(out=sm[:, 2:3], in_=sm[:, 1:2]), SM, SM)
                    T.op("dve", lambda e: e.tensor_tensor(out=t64, in0=lg[:, 8:72].rearrange("p (g e) -> p g e", e=8),
                                                          in1=ohg.unsqueeze(2).to_broadcast([128, 8, 8]), op=ALU.mult),
                         B("p4_lg", "p4_ohg"), B("p4_t64"))
                    T.op("dve", lambda e: e.tensor_reduce(out=esel, in_=t64.rearrange("p g e -> p e g"), axis=AX.X, op=ALU.add),
                         B("p4_t64"), B("p4_esel"))
                    T.op("dve", lambda e: e.tensor_reduce(out=sm[:, 3:4], in_=esel, axis=AX.X, op=ALU.max), B("p4_esel"), SM)
                    T.op("dve", lambda e: e.tensor_scalar(out=oh1, in0=esel, scalar1=sm[:, 3:4], scalar2=None, op0=ALU.is_equal),
                         B("p4_esel", "p4_sm"), B("p4_oh1"))
                    T.op("dve", lambda e: e.scalar_tensor_tensor(out=esel2, in0=oh1, scalar=-1e30, in1=esel, op0=ALU.mult, op1=ALU.add),
                         B("p4_oh1", "p4_esel"), B("p4_esel2"))
                    T.op("dve", lambda e: e.tensor_reduce(out=sm[:, 4:5], in_=esel2, axis=AX.X, op=ALU.max), B("p4_esel2"), SM)
                    T.op("dve", lambda e: e.tensor_scalar(out=oh2, in0=esel2, scalar1=sm[:, 4:5], scalar2=None, op0=ALU.is_equal),
                         B("p4_esel2", "p4_sm"), B("p4_oh2"))
                    T.op("dve", lambda e: e.tensor_tensor(out=sm[:, 5:6], in0=sm[:, 4:5], in1=sm[:, 3:4], op=ALU.subtract), SM, SM)
                    T.op("act", lambda e: e.activation(out=sm[:, 5:6], in_=sm[:, 5:6], func=AF.Exp), SM, SM)
                    T.op("dve", lambda e: e.tensor_scalar(out=sm[:, 6:7], in0=sm[:, 5:6], scalar1=1.0, scalar2=None, op0=ALU.add), SM, SM)
                    T.op("dve", lambda e: e.reciprocal(out=sm[:, 7:8], in_=sm[:, 6:7]), SM, SM)
                    T.op("dve", lambda e: e.tensor_tensor(out=sm[:, 8:9], in0=sm[:, 5:6], in1=sm[:, 7:8], op=ALU.mult), SM, SM)
                    T.op("dve", lambda e: e.tensor_scalar(out=self.swt[:, ti, :], in0=sm[:, 7:9], scalar1=sm[:, 2:3], scalar2=None,
                                                          op0=ALU.mult), SM, (), B("swt"))
                    T.op("dve", lambda e: e.tensor_tensor(out=OH1, in0=ohg.unsqueeze(2).to_broadcast([128, 8, 8]),
                                                          in1=oh1.unsqueeze(1).to_broadcast([128, 8, 8]), op=ALU.mult),
                         B("p4_ohg", "p4_oh1"), B("p4_OH1"))
                    T.op("dve", lambda e: e.tensor_tensor(out=OH2, in0=ohg.unsqueeze(2).to_broadcast([128, 8, 8]),
                                                          in1=oh2.unsqueeze(1).to_broadcast([128, 8, 8]), op=ALU.mult),
                         B("p4_ohg", "p4_oh2"), B("p4_OH2"))
                    O1f, O2f = OH1.rearrange("p g e -> p (g e)"), OH2.rearrange("p g e -> p (g e)")
                    T.op("dve", lambda e: e.tensor_tensor(out=Mb, in0=O1f, in1=O2f, op=ALU.add), B("p4_OH1", "p4_OH2"), B("p4_Mb"))
                    T.op("pe", lambda e: e.matmul(cum_ps[:, 0, :], lhsT=Ls, rhs=Mb, start=True, stop=True), B("p4_Ls", "p4_Mb"), (), B("p4_cumps"))
                    T.op("pe", lambda e: e.matmul(cum_ps[:, 1, :], lhsT=onesb, rhs=Mb, start=True, stop=True), B("p4_onesb", "p4_Mb"), (), B("p4_cumps"))
                    T.op("dve", lambda e: e.tensor_tensor(out=rk, in0=cum_ps[:, 0, :], in1=self.runtot, op=ALU.add), B("p4_cumps", "runtot"), B("p4_rk"))
                    T.op("dve", lambda e: e.tensor_tensor(out=self.runtot, in0=self.runtot, in1=cum_ps[:, 1, :], op=ALU.add), B("p4_cumps", "runtot"), B("runtot"))
                    for kk, Of, ON in ((0, O1f, "p4_OH1"), (1, O2f, "p4_OH2")):
                        T.op("dve", lambda e, Of=Of: e.tensor_tensor(out=Mf, in0=Of, in1=rk, op=ALU.mult), B(ON, "p4_rk"), B("p4_Mf"))
                        T.op("dve", lambda e, kk=kk: e.tensor_reduce(out=self.rks[:, ti, kk:kk + 1], in_=Mf, axis=AX.X, op=ALU.add),
                             B("p4_Mf"), (), B("rks"))
                        T.op("dve", lambda e, kk=kk, Of=Of: e.tensor_copy(out=self.OHs[:, ti, kk, :], in_=Of), B(ON), (), B("OHs"))
                    T.dma("sp", self.h2_d[r0:r0 + 128, :], h2b[j], *B("h2_d", HBN))
            T.barrier()

    def p4b(self):
        nc, T, B = self.nc, self.T, self.B
        self.convert_some(10 ** 6)
        with contextlib.ExitStack() as es:
            sb = lambda *a, **k: self.sb(es, *a, **k)
            bst = sb("pb_bst", [128, NBLK])
            pidx = sb("pb_pidx", [128, 1])
            f = sb("pb_f", [128, NE])
            fi = sb("pb_fi", [128, NE], I32)
            ff = sb("pb_ff", [128, NE])
            fx = sb("pb_fx", [128, NE])
            padded = sb("pb_padded", [128, NE])
            cs = [sb("pb_cs%d" % j, [128, NE]) for j in range(2)]
            pstart = sb("pb_pstart", [128, NE])
            cmp = sb("pb_cmp", [128, NBLK, NE], BF16)
            be = sb("pb_be", [128, NBLK])
            Mf = sb("pb_Mf", [128, NE])
            dsf = sb("pb_dsf", [128, 4])
            h2t = [sb("pb_h2t%d" % j, [128, D], BF16) for j in range(2)]
            T.dma("sp", bst, self.c_bst.partition_broadcast(128), *B("pb_bst", "c_bst"))
            T.dma("sp", pidx, self.c_pidx, *B("pb_pidx", "c_pidx"))
            op = lambda fn, r, w: T.op("dve", fn, B(*r), B(*w))
            op(lambda e: e.tensor_scalar(out=f, in0=self.runtot, scalar1=float(PADR - 1), scalar2=1.0 / PADR, op0=ALU.add, op1=ALU.mult), ["runtot"], ["pb_f"])
            op(lambda e: e.tensor_copy(out=fi, in_=f), ["pb_f"], ["pb_fi"])
            op(lambda e: e.tensor_copy(out=ff, in_=fi), ["pb_fi"], ["pb_ff"])
            op(lambda e: e.tensor_tensor(out=fx, in0=ff, in1=f, op=ALU.is_gt), ["pb_ff", "pb_f"], ["pb_fx"])
            op(lambda e: e.tensor_tensor(out=ff, in0=ff, in1=fx, op=ALU.subtract), ["pb_ff", "pb_fx"], ["pb_ff"])
            op(lambda e: e.tensor_scalar(out=padded, in0=ff, scalar1=float(PADR), scalar2=None, op0=ALU.mult), ["pb_ff"], ["pb_padded"])
            op(lambda e: e.tensor_copy(out=cs[0], in_=padded), ["pb_padded"], ["pb_cs0"])
            cur = 0
            for sft in (1, 2, 4, 8, 16, 32):
                a_, b_ = cs[cur], cs[1 - cur]
                an, bn = "pb_cs%d" % cur, "pb_cs%d" % (1 - cur)
                op(lambda e, a_=a_, b_=b_, sft=sft: e.tensor_tensor(out=b_[:, sft:NE], in0=a_[:, sft:NE], in1=a_[:, 0:NE - sft], op=ALU.add), [an], [bn])
                T.op("dve", lambda e, a_=a_, b_=b_, sft=sft: e.tensor_copy(out=b_[:, 0:sft], in_=a_[:, 0:sft]), B(an), (), B(bn))
                cur = 1 - cur
            pend = cs[cur]
            PEND = "pb_cs%d" % cur
            op(lambda e: e.tensor_tensor(out=pstart, in0=pend, in1=padded, op=ALU.subtract), [PEND, "pb_padded"], ["pb_pstart"])
            op(lambda e: e.tensor_tensor(out=cmp, in0=pend.unsqueeze(1).to_broadcast([128, NBLK, NE]),
                                         in1=bst.unsqueeze(2).to_broadcast([128, NBLK, NE]), op=ALU.is_le), [PEND, "pb_bst"], ["pb_cmp"])
            op(lambda e: e.tensor_reduce(out=be, in_=cmp, axis=AX.X, op=ALU.add), ["pb_cmp"], ["pb_be"])
            op(lambda e: e.tensor_scalar(out=be, in0=be, scalar1=float(NE - 1), scalar2=128.0, op0=ALU.min, op1=ALU.mult), ["pb_be"], ["pb_be"])
            op(lambda e: e.tensor_scalar(out=be, in0=be, scalar1=pidx[:, 0:1], scalar2=None, op0=ALU.add), ["pb_be", "pb_pidx"], ["pb_be"])
            op(lambda e: e.tensor_copy(out=self.widx, in_=be), ["pb_be"], ["widx"])
            T.dma("sp", h2t[0], self.h2_d[0:128, :], *B("pb_h2t0", "h2_d"), partial=False)
            for ti in range(NB * NT):
                j = ti % 2
                if ti + 1 < NB * NT:
                    T.dma("sp", h2t[1 - j], self.h2_d[(ti + 1) * 128:(ti + 2) * 128, :], *B("pb_h2t%d" % (1 - j), "h2_d"), partial=False)
                for kk in range(2):
                    op(lambda e, kk=kk: e.tensor_tensor(out=Mf, in0=self.OHs[:, ti, kk, :], in1=pstart, op=ALU.mult), ["OHs", "pb_pstart"], ["pb_Mf"])
                    T.op("dve", lambda e, kk=kk: e.tensor_reduce(out=dsf[:, kk:kk + 1], in_=Mf, axis=AX.X, op=ALU.add), B("pb_Mf"), (), B("pb_dsf"))
                op(lambda e: e.tensor_tensor(out=dsf[:, 2:4], in0=dsf[:, 0:2], in1=self.rks[:, ti, :], op=ALU.add), ["pb_dsf", "rks"], ["pb_dsf"])
                T.op("dve", lambda e: e.tensor_copy(out=self.dest[:, ti, :], in_=dsf[:, 2:4]), B("pb_dsf"), (), B("dest"))
                for kk in range(2):
                    T.dma("pool", self.xd_d, h2t[j], T.buf("xd_d"), T.buf("pb_h2t%d" % j),
                          indirect={"out": bass.IndirectOffsetOnAxis(ap=self.dest[:, ti, kk:kk + 1], axis=0)}, extra_reads=B("dest"))
            T.barrier()

    def p5(self):
        nc, T, B = self.nc, self.T, self.B
        with contextlib.ExitStack() as es:
            sb, ps = (lambda *a, **k: self.sb(es, *a, **k)), (lambda *a, **k: self.ps(es, *a, **k))
            NWB = 3
            wg = [sb("p5_wg%d" % j, [128, 16, DE], BF16) for j in range(NWB)]
            wu = [sb("p5_wu%d" % j, [128, 16, DE], BF16) for j in range(NWB)]
            wd = [sb("p5_wd%d" % j, [128, 4, D], BF16) for j in range(NWB)]
            xg = [sb("p5_xg%d" % j, [128, D], BF16) for j in range(2)]
            xgT = [sb("p5_xgT%d" % j, [128, 16, 128], BF16) for j in range(2)]
            sg = sb("p5_sg", [128, DE])
            hid = sb("p5_hid", [128, DE], BF16)
            hT = sb("p5_hT", [128, 4, 128], BF16)
            yo = [sb("p5_yo%d" % j, [128, D], BF16) for j in range(2)]
            tp = ps("p5_tp", [128, 16, 128], BF16)
            g_ps = ps("p5_g", [128, DE])
            u_ps = ps("p5_u", [128, DE])
            tph = ps("p5_tph", [128, 4, 128], BF16)
            d_ps = [ps("p5_d%d" % j, [128, 512]) for j in range(2)]

            gq = []

            def load_w(blk, j):
                off = bass.IndirectOffsetOnAxis(ap=self.widx[:, blk:blk + 1], axis=0)
                for wt_, src, nm, sn in ((wg, self.wgb_d, "p5_wg%d" % j, "wgb_d"), (wu, self.wub_d, "p5_wu%d" % j, "wub_d"),
                                         (wd, self.wdb_d, "p5_wd%d" % j, "wdb_d")):
                    if len(gq) >= 2:
                        T.wait_bufs("pool", [T.buf(gq[-2])])
                    T.dma("pool", wt_[j].rearrange("p c n -> p (c n)"), src, T.buf(nm), T.buf(sn),
                          indirect={"in": off}, extra_reads=B("widx"), partial=False)
                    gq.append(nm)

            def load_x(blk, j):
                T.dma("sp", xg[j], self.xd_d[blk * 128:(blk + 1) * 128, :], *B("p5_xg%d" % j, "xd_d"), partial=False)

            spb = PADR // 128
            nblk = NSB * spb
            dcc = [0]

            def stage_A(blk):
                j = blk % 2
                XG, XT = "p5_xg%d" % j, "p5_xgT%d" % j
                for c in range(16):
                    T.op("pe", lambda e, c=c: e.transpose(out=tp[:, c, :], in_=xg[j][:, c * 128:(c + 1) * 128], identity=self.ident_b),
                         B(XG, "ident_b"), (), B("p5_tp"))
                for q in range(4):
                    T.op("dve", lambda e, q=q: e.tensor_copy(out=xgT[j][:, q * 4:(q + 1) * 4, :], in_=tp[:, q * 4:(q + 1) * 4, :]),
                         B("p5_tp"), (), B(XT))

            def stage_Bmm(blk):
                j = blk % 2
                wj = (blk // spb) % NWB
                XT = "p5_xgT%d" % j
                for kc in range(16):
                    T.op("pe", lambda e, kc=kc: e.matmul(g_ps, lhsT=xgT[j][:, kc, :], rhs=wg[wj][:, kc, :], start=(kc == 0), stop=(kc == 15)),
                         B(XT, "p5_wg%d" % wj), (), B("p5_g"))
                for kc in range(16):
                    T.op("pe", lambda e, kc=kc: e.matmul(u_ps, lhsT=xgT[j][:, kc, :], rhs=wu[wj][:, kc, :], start=(kc == 0), stop=(kc == 15)),
                         B(XT, "p5_wu%d" % wj), (), B("p5_u"))
                T.op("act", lambda e: e.activation(out=sg, in_=g_ps, func=AF.Silu), B("p5_g"), B("p5_sg"))
                T.op("dve", lambda e: e.tensor_tensor(out=hid, in0=sg, in1=u_ps, op=ALU.mult), B("p5_sg", "p5_u"), B("p5_hid"))

            def stage_Bt(blk):
                for c in range(4):
                    T.op("pe", lambda e, c=c: e.transpose(out=tph[:, c, :], in_=hid[:, c * 128:(c + 1) * 128], identity=self.ident_b),
                         B("p5_hid", "ident_b"), (), B("p5_tph"))
                T.op("dve", lambda e: e.tensor_copy(out=hT, in_=tph), B("p5_tph"), B("p5_hT"))

            def stage_C(blk):
                j = blk % 2
                wj = (blk // spb) % NWB
                YO = "p5_yo%d" % j
                for nq in range(4):
                    dj = dcc[0] % 2
                    dcc[0] += 1
                    for kc in range(4):
                        T.op("pe", lambda e, kc=kc, nq=nq, dj=dj: e.matmul(d_ps[dj], lhsT=hT[:, kc, :], rhs=wd[wj][:, kc, nq * 512:(nq + 1) * 512],
                                                                           start=(kc == 0), stop=(kc == 3)),
                             B("p5_hT", "p5_wd%d" % wj), (), B("p5_d%d" % dj))
                    if nq % 2 == 0:
                        T.op("act", lambda e, nq=nq, dj=dj: e.activation(out=yo[j][:, nq * 512:(nq + 1) * 512], in_=d_ps[dj], func=AF.Copy),
                             B("p5_d%d" % dj), (), B(YO))
                    else:
                        T.op("dve", lambda e, nq=nq, dj=dj: e.tensor_copy(out=yo[j][:, nq * 512:(nq + 1) * 512], in_=d_ps[dj]),
                             B("p5_d%d" % dj), (), B(YO))
                T.dma("sp", self.yd_d[blk * 128:(blk + 1) * 128, :], yo[j], *B("yd_d", YO))

            load_w(0, 0)
            load_w(1, 1)
            load_x(0, 0)
            load_x(1, 1)
            stage_A(0)
            for blk in range(nblk):
                sbi = blk // spb
                if blk + 1 < nblk:
                    stage_A(blk + 1)
                if blk + 2 < nblk:
                    load_x(blk + 2, blk % 2)
                stage_Bmm(blk)
                if blk >= 1:
                    stage_C(blk - 1)
                if blk % spb == 0 and sbi + 2 < NSB:
                    load_w(sbi + 2, (sbi + 2) % NWB)
                stage_Bt(blk)
            stage_C(nblk - 1)
            T.barrier()

    def p6(self):
        nc, T, B = self.nc, self.T, self.B
        with contextlib.ExitStack() as es:
            sb = lambda *a, **k: self.sb(es, *a, **k)
            g2 = sb("p6_g2", [128, D])
            x1 = [sb("p6_x1%d" % j, [128, D]) for j in range(2)]
            ya = [sb("p6_ya%d" % j, [128, D], BF16) for j in range(2)]
            yb = [sb("p6_yb%d" % j, [128, D], BF16) for j in range(2)]
            acc = [sb("p6_acc%d" % j, [128, D]) for j in range(2)]

            def load(ti, j):
                T.dma("sp", x1[j], self.x1_d[ti * 128:(ti + 1) * 128, :], *B("p6_x1%d" % j, "x1_d"), partial=False)
                for kk, yy, nm in ((0, ya, "p6_ya%d" % j), (1, yb, "p6_yb%d" % j)):
                    T.dma("pool", yy[j], self.yd_d, T.buf(nm), T.buf("yd_d"),
                          indirect={"in": bass.IndirectOffsetOnAxis(ap=self.dest[:, ti, kk:kk + 1], axis=0)},
                          extra_reads=B("dest"), partial=False)

            load(0, 0)
            for ti in range(NB * NT):
                j = ti % 2
                b = ti // NT
                if ti % NT == 0:
                    T.dma("sp", g2, self.mod_d[b][5 * D:6 * D].partition_broadcast(128), *B("p6_g2", "mod_d"), partial=False)
                if ti + 1 < NB * NT:
                    load(ti + 1, (ti + 1) % 2)
                AN = "p6_acc%d" % j
                T.op("act", lambda e: e.activation(out=acc[j], in_=ya[j], func=AF.Copy, scale=self.swt[:, ti, 0:1]),
                     B("p6_ya%d" % j, "swt"), B(AN))
                T.op("dve", lambda e: e.scalar_tensor_tensor(out=acc[j], in0=yb[j], scalar=self.swt[:, ti, 1:2], in1=acc[j],
                                                             op0=ALU.mult, op1=ALU.add), B("p6_yb%d" % j, "swt", AN), B(AN))
                T.op("dve", lambda e: e.tensor_tensor(out=acc[j], in0=acc[j], in1=g2, op=ALU.mult), B(AN, "p6_g2"), B(AN))
                T.op("dve", lambda e: e.tensor_tensor(out=acc[j], in0=acc[j], in1=x1[j], op=ALU.add), B(AN, "p6_x1%d" % j), B(AN))
                T.dma("sp", self.out[ti * 128:(ti + 1) * 128, :], acc[j], *B("out", AN))
            T.barrier()

    def finish(self):
        T = self.T
        if "mod" in self.dumps:
            self.dump("mod", self.mod_d, T.buf("mod_d"), [NB, 6 * D])
        if "route" in self.dumps:
            self.dump("widx", self.widx, T.buf("widx"), [128, NBLK], I32)
            self.dump("dest", self.dest, T.buf("dest"), [128, NB * NT, 2], I32)
            self.dump("runtot", self.runtot, T.buf("runtot"), [128, NE])
            self.dump("swt", self.swt, T.buf("swt"), [128, NB * NT, 2])
        if "rope" in self.dumps:
            self.dump("cos", self.cos_t, T.buf("cos_t"), [128, NB * NT, 16])
            self.dump("sin", self.sin_t, T.buf("sin_t"), [128, NB * NT, 16])
        for nm, ap, shape, dt in (("qT", self.qT_d, [NB, 8, 128, S], BF16), ("kT", self.kT_d, [NB, 8, 128, S], BF16),
                                  ("v", self.v_d, [NB, S, 1024], BF16), ("z", self.z_d, [NB, S, 1024], BF16),
                                  ("xs", self.xs_d, [NB, S, 1024], BF16), ("bt", self.bt_d, [NB, S, 1024], BF16),
                                  ("BT", self.BT_d, [NB, 8, 128, S], BF16), ("CT", self.CT_d, [NB, 8, 128, S], BF16),
                                  ("dt", self.dt_d, [NB, S, 16], F32), ("yT", self.yT_d, [NB, 16, 128, S], BF16),
                                  ("x1", self.x1_d, [NB * S, D], F32)):
            if nm in self.dumps:
                self.dump(nm, ap, T.buf(nm + "_d"), shape, dt)
        T.barrier(final=True)


def host_consts():
    c = {}
    c["c_ident"] = np.eye(128, dtype=np.float32)
    ps_ = np.arange(128)[:, None]
    col = np.arange(19 * 128)[None, :]
    delta = col // 128 - 3
    pt = col % 128
    Dm = 128 * delta + pt - ps_
    Wm = ((Dm >= 0) & (Dm <= 128)).astype(np.float64) + ((Dm >= 0) & (Dm <= 512) & (Dm % 4 == 0)) \
        + ((Dm >= 0) & (Dm <= 2048) & (Dm % 16 == 0))
    with np.errstate(divide="ignore"):
        tb = np.where(Wm > 0, np.log(np.maximum(Wm, 1e-30)) * math.sqrt(128.0), NEG)
    c["c_tbx"] = tb.astype(np.float32)
    k = np.arange(128)
    c["c_U"] = (k[:, None] <= k[None, :]).astype(np.float32)
    c["c_SB"] = np.where(k[None, :] >= k[:, None], 0.0, NEG).astype(np.float32)
    c["c_invf"] = (500000.0 ** (-(np.arange(0, 32, 2, dtype=np.float32) / 32.0))).astype(np.float32)
    c["c_bst"] = (np.arange(NSB) * PADR).astype(np.float32)
    c["c_pidx"] = np.arange(128, dtype=np.float32).reshape(128, 1)
    return c


def host_shared(inp):
    f = lambda a: np.ascontiguousarray(a, dtype=np.float32)
    sh = {}
    sh["ada_w"] = f(inp["ada_w"][0])
    sh["ada_bT"] = f(inp["ada_b"][0].reshape(96, 128).T)
    sh["n1wT"] = f(inp["norm1_w"][0].reshape(16, 128).T)
    sh["w_in"] = f(inp["w_in"][0])
    sh["qkw"] = f(np.stack([inp["q_norm_w"][0], inp["k_norm_w"][0]]))
    sh["cwT"] = f(inp["conv_w"][0].reshape(4, 24, 128).transpose(2, 1, 0))
    sh["cbT"] = f(inp["conv_b"][0].reshape(24, 128).T)
    sh["dtb"] = f(inp["dt_bias"][0])
    sh["alog"] = f(inp["a_log"][0])
    sh["dskip"] = f(inp["d_skip"][0])
    sh["ssm_nw"] = f(inp["ssm_norm_w"][0])
    sh["w_out"] = f(inp["w_out"][0])
    sh["n2w"] = f(inp["norm2_w"][0])
    sh["wr"] = f(np.concatenate([inp["router_group_w"][0], inp["router_expert_w"][0]], axis=1))
    sh["br"] = f(np.concatenate([inp["router_group_b"][0], inp["router_expert_b"][0]]))
    sh["w_gate"] = f(inp["w_gate"][0].reshape(NE, 16, 128, DE).transpose(0, 2, 1, 3).reshape(NE * 128, 16 * DE))
    sh["w_up"] = f(inp["w_up"][0].reshape(NE, 16, 128, DE).transpose(0, 2, 1, 3).reshape(NE * 128, 16 * DE))
    sh["w_down"] = f(inp["w_down"][0].reshape(NE, 4, 128, D).transpose(0, 2, 1, 3).reshape(NE * 128, 4 * D))
    sh.update(host_consts())
    return sh


def host_core(inp, core):
    b0 = core * NB
    m = {}
    m["x"] = np.ascontiguousarray(inp["x"][b0:b0 + NB].reshape(NB * S, D), dtype=np.float32)
    c2 = np.asarray(inp["c"][b0:b0 + NB], dtype=np.float32)
    m["cT"] = np.ascontiguousarray(c2.reshape(NB, 16, 128).transpose(2, 1, 0))
    p2 = np.asarray(inp["positions"][b0:b0 + NB], dtype=np.int32)
    m["pos"] = np.ascontiguousarray(p2.reshape(NB * NT, 128).T)
    return m


def build(stop_after=None, dumps=()):
    k = K(stop_after=stop_after)
    k.declare()
    k.dumps = set(dumps)
    phases = [("p0", k.p0)]
    for nm in ("p1", "p2", "p3", "p4", "p4b", "p5", "p6"):
        if hasattr(k, nm):
            phases.append((nm, getattr(k, nm)))
    for nm, fn in phases:
        fn()
        if stop_after is not None and stop_after == nm or (stop_after or "").startswith("p1") and nm == "p1":
            break
    k.finish()
    return k


def kernel(**inputs):
    k = build()
    sh = host_shared(inputs)
    in_maps = []
    for c in range(NCORES):
        m = dict(sh)
        m.update(host_core(inputs, c))
        in_maps.append(m)
    res = run_bass_kernel_spmd(k.nc, in_maps, core_ids=list(range(NCORES)))
    outs = [np.asarray(r["out"]).reshape(NB, S, D) for r in res.results]
    return np.concatenate(outs, axis=0).astype(np.float32)
```

```python
import contextlib
import math
import numpy as np
import concourse.bass as bass
import concourse.mybir as mybir
from concourse.bass_utils import run_bass_kernel_spmd

F32 = mybir.dt.float32
BF16 = mybir.dt.bfloat16
I32 = mybir.dt.int32
AF = mybir.ActivationFunctionType
ALU = mybir.AluOpType
AX = mybir.AxisListType

NCORES = 8
D = 2048
S = 2048
NB = 2
NT = S // 128
DIN = 7184
NE = 64
PADR = 256
NSB = 96
NBLK = NSB
NROWS = NSB * PADR
DE = 512
EPS = 1e-6
NEG = -30000.0


class Buf:
    __slots__ = ("name", "w", "r", "pre", "sems")

    def __init__(self, name):
        self.name = name
        self.w = {}
        self.r = {}
        self.pre = {}
        self.sems = {}


def _add(d, tok):
    k = id(tok[0])
    o = d.get(k)
    if o is None or o[1] < tok[1]:
        d[k] = tok


class Trk:
    def __init__(self, nc, same_engine_sync=True):
        self.nc = nc
        self.same = same_engine_sync
        self.eng = {}
        for nm, h in (("pe", nc.tensor), ("act", nc.scalar), ("dve", nc.vector),
                      ("pool", nc.gpsimd), ("sp", nc.sync)):
            self.eng[nm] = dict(h=h, sem=nc.alloc_semaphore(name="s_" + nm), cnt=0, waited={}, name=nm)
        self.bufs = {}
        self.record = None
        self.bg = set()
        self.dmasems = {}
        self.nins = 0

    def buf(self, name):
        b = self.bufs.get(name)
        if b is None:
            b = Buf(name)
            self.bufs[name] = b
        return b

    def _wait(self, e, deps):
        for sem, val in deps:
            if sem is e["sem"] and (not self.same or e["name"] == "pe"):
                continue
            k = id(sem)
            db = self.dmasems.get(k)
            if db is not None:
                val = db[1]
            if e["waited"].get(k, 0) >= val:
                continue
            e["h"].wait_ge(sem, val)
            e["waited"][k] = val

    def _deps(self, reads, writes, pwrites):
        deps = []
        for b in reads:
            deps.extend(b.w.values())
        for b in writes:
            deps.extend(b.w.values())
            deps.extend(b.r.values())
        for b in pwrites:
            if b.r:
                pre = dict(b.w)
                for t in b.r.values():
                    _add(pre, t)
                b.pre, b.w, b.r = pre, {}, {}
            deps.extend(b.pre.values())
        return deps

    def _commit(self, tok, reads, writes, pwrites):
        for b in writes:
            b.w = {id(tok[0]): tok}
            b.r = {}
            b.pre = {}
        for b in pwrites:
            _add(b.w, tok)
        for b in reads:
            _add(b.r, tok)

    def mark(self):
        if self.record is not None:
            self.record.append(None)

    def emit_interleaved(self, la, lb):
        ia = ib = 0
        while ia < len(la) or ib < len(lb):
            if ia < len(la):
                la[ia]()
                ia += 1
            if ib < len(lb):
                lb[ib]()
                ib += 1

    def op(self, eng, fn, reads=(), writes=(), pwrites=()):
        if self.record is not None:
            self.record.append(lambda: self.op(eng, fn, reads, writes, pwrites))
            return
        e = self.eng[eng]
        self._wait(e, self._deps(reads, writes, pwrites))
        ins = fn(e["h"])
        e["cnt"] += 1
        ins.then_inc(e["sem"], 1)
        self.nins += 1
        self._commit((e["sem"], e["cnt"]), reads, writes, pwrites)

    def dma(self, q, out, in_, dst, src, indirect=None, extra_reads=(), partial=True, **kw):
        if self.record is not None:
            rec = self.record
            rec.append(lambda: self._dma_now(q, out, in_, dst, src, indirect, extra_reads, partial, kw))
            return
        self._dma_now(q, out, in_, dst, src, indirect, extra_reads, partial, kw)

    def _dma_now(self, q, out, in_, dst, src, indirect, extra_reads, partial, kw):
        e = self.eng[q]
        reads = [src] + list(extra_reads)
        writes, pwrites = ([], [dst]) if partial else ([dst], [])
        self._wait(e, self._deps(reads, writes, pwrites))
        kind = "sw" if q == "pool" else "hw"
        ent = dst.sems.get(kind)
        if ent is None:
            ent = [self.nc.alloc_semaphore(name="d%s_%s" % (kind, dst.name)), 0]
            dst.sems[kind] = ent
            self.dmasems[id(ent[0])] = ent
        if indirect is not None:
            ins = e["h"].indirect_dma_start(out, indirect.get("out"), in_, indirect.get("in"), **kw)
        else:
            ins = e["h"].dma_start(out=out, in_=in_, **kw)
        ent[1] += 16
        ins.then_inc(ent[0], 16)
        self.nins += 1
        self._commit((ent[0], ent[1]), reads, writes, pwrites)

    def barrier(self, final=False):
        bgs = set() if final else {id(ent[0]) for n in self.bg for ent in self.buf(n).sems.values()}
        toks = [(e["sem"], e["cnt"]) for e in self.eng.values() if e["cnt"] > 0]
        toks += [(ent[0], ent[1]) for ent in self.dmasems.values() if ent[1] > 0 and id(ent[0]) not in bgs]
        for e in self.eng.values():
            self._wait(e, toks)
        for b in self.bufs.values():
            if final or b.name not in self.bg:
                b.w, b.r, b.pre = {}, {}, {}

    def wait_bufs(self, q, bufs):
        if self.record is not None:
            rec = self.record
            rec.append(lambda: self._wait_bufs_now(q, bufs))
            return
        self._wait_bufs_now(q, bufs)

    def _wait_bufs_now(self, q, bufs):
        e = self.eng[q]
        deps = []
        for b in bufs:
            deps.extend(b.w.values())
        self._wait(e, deps)


class K:
    def __init__(self, stop_after=None, debug=False):
        self.stop_after = stop_after
        self.debug = debug
        nc = self.nc = bass.Bass("TRN2", target_bir_lowering=False)
        self.T = Trk(nc)
        self.din = {}
        self.dbg = {}

    def inp(self, name, shape, dt=F32):
        t = self.nc.dram_tensor(name, list(shape), dt, kind="ExternalInput").ap()
        self.din[name] = t
        self.T.buf(name)
        return t

    def scratch(self, name, shape, dt):
        t = self.nc.dram_tensor(name, list(shape), dt, kind="Internal").ap()
        self.T.buf(name)
        return t

    def outp(self, name, shape, dt=F32):
        t = self.nc.dram_tensor(name, list(shape), dt, kind="ExternalOutput").ap()
        self.T.buf(name)
        return t

    def sb(self, es, name, shape, dt=F32):
        self.uid = getattr(self, "uid", 0) + 1
        h = es.enter_context(self.nc.sbuf_tensor("%s_u%d" % (name, self.uid), list(shape), dt))
        return h.ap()

    def ps(self, es, name, shape, dt=F32):
        self.uid = getattr(self, "uid", 0) + 1
        h = es.enter_context(self.nc.psum_tensor("%s_u%d" % (name, self.uid), list(shape), dt))
        return h.ap()

    def B(self, *names):
        return [self.T.buf(n) for n in names]

    def convert_some(self, k):
        for _ in range(k):
            if self.cq:
                d_, s_, dn, sn = self.cq.pop()
                self.T.dma("pool", d_, s_, self.T.buf(dn), self.T.buf(sn))

    def dump(self, name, src_ap, src_buf, shape, dt=F32):
        o = self.outp("dbg_" + name, shape, dt)
        self.T.dma("sp", o, src_ap, self.T.buf("dbg_" + name), src_buf)
        self.dbg[name] = o

    def declare(self):
        nc = self.nc
        i = self.inp
        self.x = i("x", [NB * S, D])
        self.cT = i("cT", [128, 16, NB])
        self.pos = i("pos", [128, NB * NT], I32)
        self.ada_w = i("ada_w", [D, 6 * D])
        self.ada_bT = i("ada_bT", [128, 96])
        self.n1wT = i("n1wT", [128, 16])
        self.w_in = i("w_in", [D, DIN])
        self.qkw = i("qkw", [2, 128])
        self.cwT = i("cwT", [128, 24, 4])
        self.cbT = i("cbT", [128, 24])
        self.dtb = i("dtb", [16])
        self.alog = i("alog", [16])
        self.dskip = i("dskip", [16])
        self.ssm_nw = i("ssm_nw", [1024])
        self.w_out = i("w_out", [D, D])
        self.n2w = i("n2w", [D])
        self.wr = i("wr", [D, 72])
        self.br = i("br", [72])
        self.w_gate = i("w_gate", [NE * 128, 16 * DE])
        self.w_up = i("w_up", [NE * 128, 16 * DE])
        self.w_down = i("w_down", [NE * 128, 4 * D])
        self.c_ident = i("c_ident", [128, 128])
        self.c_tbx = i("c_tbx", [128, 19 * 128])
        self.c_U = i("c_U", [128, 128])
        self.c_SB = i("c_SB", [128, 128])
        self.c_invf = i("c_invf", [16])
        self.c_bst = i("c_bst", [NSB])
        self.c_pidx = i("c_pidx", [128, 1])
        self.out = self.outp("out", [NB * S, D])
        s = self.scratch
        self.mod_d = s("mod_d", [NB, 6 * D], F32)
        self.qT_d = s("qT_d", [NB, 8, 128, S], BF16)
        self.kT_d = s("kT_d", [NB, 8, 128, S], BF16)
        self.v_d = s("v_d", [NB, S, 1024], BF16)
        self.z_d = s("z_d", [NB, S, 1024], BF16)
        self.xs_d = s("xs_d", [NB, S, 1024], BF16)
        self.bt_d = s("bt_d", [NB, S, 1024], BF16)
        self.BT_d = s("BT_d", [NB, 8, 128, S], BF16)
        self.CT_d = s("CT_d", [NB, 8, 128, S], BF16)
        self.dt_d = s("dt_d", [NB, S, 16], F32)
        self.yT_d = s("yT_d", [NB, 16, 128, S], BF16)
        self.x1_d = s("x1_d", [NB * S, D], F32)
        self.h2_d = s("h2_d", [NB * S, D], BF16)
        self.wgb_d = s("wgb_d", [NE * 128, 16 * DE], BF16)
        self.wub_d = s("wub_d", [NE * 128, 16 * DE], BF16)
        self.wdb_d = s("wdb_d", [NE * 128, 4 * D], BF16)
        self.xd_d = s("xd_d", [NROWS, D], BF16)
        self.yd_d = s("yd_d", [NROWS, D], BF16)
        self.pes = contextlib.ExitStack()
        es = self.pes
        self.ident_f = self.sb(es, "ident_f", [128, 128], F32)
        self.ident_b = self.sb(es, "ident_b", [128, 128], BF16)
        self.A1T = self.sb(es, "A1T", [128, NB, 16], F32)
        self.S1T = self.sb(es, "S1T", [128, NB, 16], F32)
        self.cos_t = self.sb(es, "cos_t", [128, NB * NT, 16], F32)
        self.sin_t = self.sb(es, "sin_t", [128, NB * NT, 16], F32)
        self.dest = self.sb(es, "dest", [128, NB * NT, 2], I32)
        self.swt = self.sb(es, "swt", [128, NB * NT, 2], F32)
        self.OHs = self.sb(es, "OHs", [128, NB * NT, 2, NE], BF16)
        self.rks = self.sb(es, "rks", [128, NB * NT, 2], F32)
        self.runtot = self.sb(es, "runtot", [128, NE], F32)
        self.widx = self.sb(es, "widx", [128, NBLK], I32)

    def p0(self):
        nc, T = self.nc, self.T
        B = self.B
        with contextlib.ExitStack() as es:
            cT = self.sb(es, "p0_cT", [128, 16, NB])
            cact = self.sb(es, "p0_cact", [128, 16, NB], BF16)
            abT = self.sb(es, "p0_abT", [128, 96])
            n1wT = self.sb(es, "p0_n1wT", [128, 16])
            modT = self.sb(es, "p0_modT", [128, NB, 96])
            modrow = self.sb(es, "p0_modrow", [96, 128])
            wb = [self.sb(es, "p0_wb%d" % j, [128, 16, 512], BF16) for j in range(2)]
            posi = self.sb(es, "p0_posi", [128, NB * NT], I32)
            posf = self.sb(es, "p0_posf", [128, NB * NT])
            invf = self.sb(es, "p0_invf", [128, 16])
            ang = self.sb(es, "p0_ang", [128, NB * NT, 16])
            a1 = self.sb(es, "p0_a1", [128, NB * NT, 16])
            modps = self.ps(es, "p0_modps", [128, NB, 96])
            tps = self.ps(es, "p0_tps", [96, 128])
            T.dma("sp", self.ident_f, self.c_ident, *B("ident_f", "c_ident"))
            T.dma("pool", self.ident_b, self.c_ident, *B("ident_b", "c_ident"))
            T.dma("sp", cT, self.cT, *B("p0_cT", "cT"))
            T.dma("sp", abT, self.ada_bT, *B("p0_abT", "ada_bT"))
            T.dma("sp", n1wT, self.n1wT, *B("p0_n1wT", "n1wT"))
            T.dma("sp", posi, self.pos, *B("p0_posi", "pos"))
            T.dma("sp", invf, self.c_invf.partition_broadcast(128), *B("p0_invf", "c_invf"))
            T.op("act", lambda e: e.activation(out=cact, in_=cT, func=AF.Silu), B("p0_cT"), B("p0_cact"))
            T.op("dve", lambda e: e.tensor_copy(out=posf, in_=posi), B("p0_posi"), B("p0_posf"))
            sh3 = [128, NB * NT, 16]
            T.op("dve", lambda e: e.tensor_tensor(out=ang, in0=posf.unsqueeze(2).to_broadcast(sh3),
                                                  in1=invf.unsqueeze(1).to_broadcast(sh3), op=ALU.mult),
                 B("p0_posf", "p0_invf"), B("p0_ang"))
            ui = self.sb(es, "p0_ui", [128, NB * NT, 16], I32)
            uf = self.sb(es, "p0_uf", [128, NB * NT, 16])
            for tab, off, tn in ((self.sin_t, 0.5, "sin_t"), (self.cos_t, 0.75, "cos_t")):
                T.op("dve", lambda e, off=off: e.tensor_scalar(out=a1, in0=ang, scalar1=1.0 / (2.0 * math.pi), scalar2=off,
                                                               op0=ALU.mult, op1=ALU.add), B("p0_ang"), B("p0_a1"))
                T.op("dve", lambda e: e.tensor_copy(out=ui, in_=a1), B("p0_a1"), B("p0_ui"))
                T.op("dve", lambda e: e.tensor_copy(out=uf, in_=ui), B("p0_ui"), B("p0_uf"))
                T.op("dve", lambda e: e.tensor_tensor(out=a1, in0=a1, in1=uf, op=ALU.subtract), B("p0_a1", "p0_uf"), B("p0_a1"))
                T.op("dve", lambda e: e.tensor_scalar(out=uf, in0=a1, scalar1=0.0, scalar2=None, op0=ALU.is_lt),
                     B("p0_a1"), B("p0_uf"))
                T.op("dve", lambda e: e.tensor_tensor(out=a1, in0=a1, in1=uf, op=ALU.add), B("p0_a1", "p0_uf"), B("p0_a1"))
                T.op("act", lambda e, tab=tab: e.activation(out=tab, in_=a1, func=AF.Sin, scale=2.0 * math.pi, bias=-math.pi),
                     B("p0_a1"), B(tn))
            wsrc = self.ada_w.rearrange("(c p) n -> p c n", p=128)
            for n in range(24):
                w = wb[n % 2]
                wn = "p0_wb%d" % (n % 2)
                T.dma("pool", w, wsrc[:, :, n * 512:(n + 1) * 512], *B(wn, "ada_w"), partial=False)
                for m in range(4):
                    for k in range(16):
                        T.op("pe", lambda e, w=w, m=m, k=k, n=n: e.matmul(
                            modps[:, :, n * 4 + m], lhsT=w[:, k, m * 128:(m + 1) * 128], rhs=cact[:, k, :],
                            start=(k == 0), stop=(k == 15)), B(wn, "p0_cact"), (), B("p0_modps"))
            T.op("dve", lambda e: e.tensor_tensor(out=modT, in0=modps, in1=abT.unsqueeze(1).to_broadcast([128, NB, 96]),
                                                  op=ALU.add), B("p0_modps", "p0_abT"), B("p0_modT"))
            for b in range(NB):
                T.op("dve", lambda e, b=b: e.scalar_tensor_tensor(out=self.A1T[:, b, :], in0=modT[:, b, 16:32], scalar=1.0,
                                                                  in1=n1wT, op0=ALU.add, op1=ALU.mult),
                     B("p0_modT", "p0_n1wT"), (), B("A1T"))
                T.op("dve", lambda e, b=b: e.tensor_copy(out=self.S1T[:, b, :], in_=modT[:, b, 0:16]),
                     B("p0_modT"), (), B("S1T"))
                T.op("pe", lambda e, b=b: e.transpose(out=tps, in_=modT[:, b, :], identity=self.ident_f),
                     B("p0_modT", "ident_f"), B("p0_tps"))
                T.op("dve", lambda e: e.tensor_copy(out=modrow, in_=tps), B("p0_tps"), B("p0_modrow"))
                T.dma("sp", self.mod_d[b].rearrange("(j p) -> j p", p=128), modrow, *B("mod_d", "p0_modrow"))
            T.barrier()

    def p1(self):
        T, B = self.T, self.B
        zt = self.sb(self.pes, "zt", [128, 2048], BF16)
        T.op("pool", lambda e: e.memset(zt, 0.0), (), B("zt"))
        per = NROWS // 128 * D
        xdv = self.xd_d.rearrange("(p r) d -> p (r d)", p=128)
        self.zq = [(xdv[:, q * 2048:(q + 1) * 2048], zt) for q in range(per // 2048)]
        self.cq = []
        T.bg = {"wgb_d", "wub_d", "wdb_d"}
        for e_ in range(NE):
            for dst, src, dn, sn in ((self.wgb_d, self.w_gate, "wgb_d", "w_gate"), (self.wub_d, self.w_up, "wub_d", "w_up"),
                                     (self.wdb_d, self.w_down, "wdb_d", "w_down")):
                self.cq.append((dst[e_ * 128:(e_ + 1) * 128, :], src[e_ * 128:(e_ + 1) * 128, :], dn, sn))
        self.cq.reverse()
        for b in range(NB):
            self.p1_batch(b)

    def p1_batch(self, b):
        nc, T, B = self.nc, self.T, self.B
        with contextlib.ExitStack() as es:
            sb, ps = (lambda *a, **k: self.sb(es, *a, **k)), (lambda *a, **k: self.ps(es, *a, **k))
            hT = sb("p1_hT", [128, 16, S], BF16)
            xt = [sb("p1_xt%d" % j, [128, D]) for j in range(2)]
            xn = [sb("p1_xn%d" % j, [128, D], BF16) for j in range(2)]
            junk = sb("p1_junk", [128, D], BF16)
            st = [sb("p1_st%d" % j, [128, 8]) for j in range(2)]
            wb = [sb("p1_wb%d" % j, [128, 16, 512], BF16) for j in range(2)]
            stage = sb("p1_stage", [128, NT, 512], BF16)
            qTs = sb("p1_qTs", [128, 4, S], BF16)
            xr = sb("p1_xr", [128, 3 + S])
            acc = sb("p1_acc", [128, S])
            cv = sb("p1_cv", [128, S], BF16)
            sq = sb("p1_sq", [128, 8, 128], BF16)
            qn = sb("p1_qn", [128, 8, 128])
            qb = sb("p1_qb", [128, 8, 128], BF16)
            s4 = sb("p1_s4", [128, 16])
            s4b_tile = sb("p1_s4b", [128, 16])
            rr = [sb("p1_rr%d" % j, [128, 2, 4, 16]) for j in range(4)]
            qkw = sb("p1_qkw", [128, 2, 128])
            cwT = sb("p1_cwT", [128, 24, 4])
            cbT = sb("p1_cbT", [128, 24])
            dtb = sb("p1_dtb", [128, 16])
            dtt = sb("p1_dtt", [128, 16])
            dts = sb("p1_dts", [128, NT, 16])
            tp = [ps("p1_tp%d" % j, [128, 16, 128], BF16) for j in range(2)]
            mm_all = ps("p1_mm", [128, 4, 512])
            mm = [mm_all[:, j, :] for j in range(4)]
            T.dma("sp", qkw, self.qkw.rearrange("a e -> (a e)").partition_broadcast(128), *B("p1_qkw", "qkw"))
            T.dma("sp", cwT, self.cwT, *B("p1_cwT", "cwT"))
            T.dma("sp", cbT, self.cbT, *B("p1_cbT", "cbT"))
            T.dma("sp", dtb, self.dtb.partition_broadcast(128), *B("p1_dtb", "dtb"))
            T.op("pool", lambda e: e.memset(xr[:, 0:3], 0.0), (), (), B("p1_xr"))
            wsrc = self.w_in.rearrange("(c p) n -> p c n", p=128)

            def load_w(n):
                c0, c1 = n * 512, min((n + 1) * 512, DIN)
                T.dma("pool", wb[n % 2][:, :, 0:c1 - c0], wsrc[:, :, c0:c1], *B("p1_wb%d" % (n % 2), "w_in"), partial=False)

            load_w(0)
            def load_x(i):
                r0 = b * S + i * 128
                T.dma("sp", xt[i % 2], self.x[r0:r0 + 128, :], *B("p1_xt%d" % (i % 2), "x"), partial=False)

            load_x(0)

            def stats(i):
                j = i % 2
                if i + 1 < NT:
                    load_x(i + 1)
                for _ in range(4):
                    if self.zq:
                        zo, zi = self.zq.pop()
                        T.dma("sp", zo, zi, *B("xd_d", "zt"))
                X, XN, ST = "p1_xt%d" % j, "p1_xn%d" % j, "p1_st%d" % j
                T.op("dve", lambda e: e.memset(st[j][:, 0:1], 0.0), (), B(ST))
                T.op("act", lambda e: e.activation(out=junk, in_=xt[j], func=AF.Square, accum_out=st[j][:, 0:1]),
                     B(X), B("p1_junk", ST))
                T.op("dve", lambda e: e.tensor_scalar(out=st[j][:, 1:2], in0=st[j][:, 0:1], scalar1=1.0 / D, scalar2=EPS,
                                                      op0=ALU.mult, op1=ALU.add), B(ST), B(ST))
                T.op("act", lambda e: e.activation(out=st[j][:, 2:3], in_=st[j][:, 1:2], func=AF.Sqrt), B(ST), B(ST))
                T.op("dve", lambda e: e.reciprocal(out=st[j][:, 3:4], in_=st[j][:, 2:3]), B(ST), B(ST))
                T.op("act", lambda e: e.activation(out=xn[j], in_=xt[j], func=AF.Copy, scale=st[j][:, 3:4]),
                     B(X, ST), B(XN))

            def tev(i):
                j = i % 2
                XN, TP = "p1_xn%d" % j, "p1_tp%d" % j
                for c in range(16):
                    T.op("pe", lambda e, c=c: e.transpose(out=tp[j][:, c, :], in_=xn[j][:, c * 128:(c + 1) * 128],
                                                          identity=self.ident_b),
                         B(XN, "ident_b"), (), B(TP))
                for c in range(16):
                    o = hT[:, c, i * 128:(i + 1) * 128]
                    T.op("dve", lambda e, c=c, o=o: e.tensor_scalar(out=o, in0=tp[j][:, c, :], scalar1=self.A1T[:, b, c:c + 1],
                                                                    scalar2=self.S1T[:, b, c:c + 1], op0=ALU.mult, op1=ALU.add),
                         B(TP, "A1T", "S1T"), (), B("p1_hT%d" % i))

            stats(0)
            for i in range(NT):
                if i + 1 < NT:
                    stats(i + 1)
                tev(i)
            if "hT" in self.dumps and b == 0:
                for i in range(1, NT):
                    T.wait_bufs("sp", B("p1_hT%d" % i))
                self.dump("hT", hT, T.buf("p1_hT0"), [128, 16, S], BF16)
            tpc = [0]
            nblk = {"p1a": 0, "p1q": 2, "p1v": 6, "p1x": 9}.get(self.stop_after, 15)
            for n in range(nblk):
                if n + 1 < 15:
                    load_w(n + 1)
                self.convert_some(3)
                for _ in range(2):
                    if self.zq:
                        zo, zi = self.zq.pop()
                        T.dma("sp", zo, zi, *B("xd_d", "zt"))
                w = wb[n % 2]
                WN = "p1_wb%d" % (n % 2)
                hh = n % 2
                if n < 4:
                    which = 0 if n < 2 else 1
                    s4b = s4b_tile
                    csets = [
                        dict(sq=sq, qn=qn, qb=qb, rr=rr, s4=s4, SQ="p1_sq", QN="p1_qn", QB="p1_qb",
                             RR=["p1_rr0", "p1_rr1", "p1_rr2", "p1_rr3"], S4="p1_s4"),
                        dict(sq=stage[:, 0:2, :].rearrange("p t (h e) -> p (t h) e", e=128),
                             qn=acc[:, 0:1024].rearrange("p (g e) -> p g e", e=128),
                             qb=stage[:, 2:4, :].rearrange("p t (h e) -> p (t h) e", e=128),
                             rr=[xr[:, 3 + 128 * q_:3 + 128 * (q_ + 1)].rearrange("p (t h e) -> p t h e", t=2, h=4) for q_ in range(4)],
                             s4=s4b, SQ="p1_stage", QN="p1_acc", QB="p1_stage", RR=["p1_xr"] * 4, S4="p1_s4b"),
                    ]
                    def mm_pair(a2):
                        i0 = 2 * a2
                        k0 = i0 % 4
                        for t2 in range(2):
                            i = i0 + t2
                            for kc in range(16):
                                T.op("pe", lambda e, kc=kc, i=i, t2=t2: e.matmul(mm[k0 + t2], lhsT=hT[:, kc, i * 128:(i + 1) * 128], rhs=w[:, kc, :],
                                                                                 start=(kc == 0), stop=(kc == 15)),
                                     B("p1_hT%d" % i, WN), (), B("p1_mm%d" % (k0 + t2)))

                    def chain_pre(a2):
                        cs = csets[a2 % 2]
                        sq_, qn_, qb_, rr_, s4_ = cs["sq"], cs["qn"], cs["qb"], cs["rr"], cs["s4"]
                        SQ, QN, QB, RR, S4 = cs["SQ"], cs["QN"], cs["QB"], cs["RR"], cs["S4"]
                        i0 = 2 * a2
                        k0 = i0 % 4
                        MNs = ["p1_mm%d" % k0, "p1_mm%d" % (k0 + 1)]
                        M8 = mm_all[:, k0:k0 + 2, :].rearrange("p t (h e) -> p (t h) e", e=128)
                        T.op("act", lambda e: e.activation(out=sq_, in_=M8, func=AF.Square), B(*MNs), B(SQ))
                        T.op("dve", lambda e: e.tensor_reduce(out=s4_[:, 0:8], in_=sq_, axis=AX.X, op=ALU.add), B(SQ), B(S4))
                        T.op("dve", lambda e: e.tensor_scalar(out=s4_[:, 0:8], in0=s4_[:, 0:8], scalar1=1.0 / 128, scalar2=EPS,
                                                              op0=ALU.mult, op1=ALU.add), B(S4), B(S4))
                        T.op("act", lambda e: e.activation(out=s4_[:, 0:8], in_=s4_[:, 0:8], func=AF.Sqrt), B(S4), B(S4))
                        T.op("dve", lambda e: e.reciprocal(out=s4_[:, 8:16], in_=s4_[:, 0:8]), B(S4), B(S4))
                        T.op("dve", lambda e: e.tensor_tensor(out=qn_, in0=M8, in1=s4_[:, 8:16].unsqueeze(2).to_broadcast([128, 8, 128]),
                                                              op=ALU.mult), B(S4, *MNs), B(QN))
                        T.op("dve", lambda e: e.tensor_tensor(out=qn_, in0=qn_,
                                                              in1=qkw[:, which, :].unsqueeze(1).to_broadcast([128, 8, 128]),
                                                              op=ALU.mult), B(QN, "p1_qkw"), B(QN))
                        ti = b * NT + i0
                        sh4 = [128, 2, 4, 16]
                        cosb = self.cos_t[:, ti:ti + 2, :].unsqueeze(2).to_broadcast(sh4)
                        sinb = self.sin_t[:, ti:ti + 2, :].unsqueeze(2).to_broadcast(sh4)
                        qn4 = qn_.rearrange("p (t h) e -> p t h e", t=2)
                        t1, t2_ = qn4[:, :, :, 0:16], qn4[:, :, :, 16:32]
                        T.op("dve", lambda e: e.tensor_tensor(out=rr_[0], in0=t1, in1=cosb, op=ALU.mult), B(QN, "cos_t"), (), B(RR[0]))
                        T.op("dve", lambda e: e.tensor_tensor(out=rr_[1], in0=t2_, in1=sinb, op=ALU.mult), B(QN, "sin_t"), (), B(RR[1]))
                        T.op("dve", lambda e: e.tensor_tensor(out=rr_[2], in0=t2_, in1=cosb, op=ALU.mult), B(QN, "cos_t"), (), B(RR[2]))
                        T.op("dve", lambda e: e.tensor_tensor(out=rr_[3], in0=t1, in1=sinb, op=ALU.mult), B(QN, "sin_t"), (), B(RR[3]))
                        T.op("dve", lambda e: e.tensor_tensor(out=t1, in0=rr_[0], in1=rr_[1], op=ALU.subtract),
                             B(RR[0], RR[1]), B(QN))
                        T.op("dve", lambda e: e.tensor_tensor(out=t2_, in0=rr_[2], in1=rr_[3], op=ALU.add),
                             B(RR[2], RR[3]), B(QN))
                        T.op("act", lambda e: e.activation(out=qb_, in_=qn_, func=AF.Copy), B(QN), B(QB))

                    def chain_post(a2):
                        cs = csets[a2 % 2]
                        qb_, QB = cs["qb"], cs["QB"]
                        i0 = 2 * a2
                        tj = tpc[0] % 2
                        tpc[0] += 1
                        TP = "p1_tp%d" % tj
                        for th in range(8):
                            T.op("pe", lambda e, th=th: e.transpose(out=tp[tj][:, th, :], in_=qb_[:, th, :],
                                                                    identity=self.ident_b), B(QB, "ident_b"), (), B(TP))
                        T.op("dve", lambda e: e.tensor_copy(out=qTs[:, :, i0 * 128:(i0 + 2) * 128].rearrange("p h (t e) -> p h t e", t=2),
                                                            in_=tp[tj][:, 0:8, :].rearrange("p (t h) e -> p h t e", t=2)),
                             B(TP), (), B("p1_qTs"))

                    npair = NT // 2
                    mm_pair(0)
                    mm_pair(1)
                    chain_pre(0)
                    for a2 in range(npair):
                        if a2 + 1 < npair:
                            chain_pre(a2 + 1)
                        if a2 + 2 < npair:
                            mm_pair(a2 + 2)
                        chain_post(a2)
                    dst = self.qT_d if which == 0 else self.kT_d
                    dn = "qT_d" if which == 0 else "kT_d"
                    T.dma("sp", dst[b, hh * 4:(hh + 1) * 4].rearrange("h e t -> e h t"), qTs, *B(dn, "p1_qTs"))
                elif n < 8:
                    for i in range(NT):
                        M = mm[i % 4]
                        MN = "p1_mm%d" % (i % 4)
                        for kc in range(16):
                            T.op("pe", lambda e, kc=kc: e.matmul(M, lhsT=hT[:, kc, i * 128:(i + 1) * 128], rhs=w[:, kc, :],
                                                                 start=(kc == 0), stop=(kc == 15)),
                                 B("p1_hT%d" % i, WN), (), B(MN))
                        if i % 2 == 0:
                            T.op("act", lambda e: e.activation(out=stage[:, i, :], in_=M, func=AF.Copy), B(MN), (), B("p1_stage"))
                        else:
                            T.op("dve", lambda e: e.tensor_copy(out=stage[:, i, :], in_=M), B(MN), (), B("p1_stage"))
                    dst, dn = (self.v_d, "v_d") if n < 6 else (self.z_d, "z_d")
                    T.dma("sp", dst[b].rearrange("(i p) c -> p i c", p=128)[:, :, hh * 512:(hh + 1) * 512], stage,
                          *B(dn, "p1_stage"))
                elif n < 14:
                    cvs = [(cv, "p1_cv"), (junk, "p1_junk")]

                    def st1(m):
                        ch = (n - 8) * 4 + m
                        cvt, CVN = cvs[m % 2]
                        for tg in range(4):
                            M = mm[tg]
                            MN = "p1_mm%d" % tg
                            for kc in range(16):
                                T.op("pe", lambda e, kc=kc, M=M, tg=tg: e.matmul(
                                    M, lhsT=w[:, kc, m * 128:(m + 1) * 128], rhs=hT[:, kc, tg * 512:(tg + 1) * 512],
                                    start=(kc == 0), stop=(kc == 15)),
                                     B(WN, *["p1_hT%d" % (tg * 4 + q) for q in range(4)]), (), B(MN))
                            T.op("act", lambda e, M=M, tg=tg: e.activation(out=xr[:, 3 + tg * 512:3 + (tg + 1) * 512], in_=M, func=AF.Copy),
                                 B(MN), (), B("p1_xr"))
                        T.op("dve", lambda e: e.tensor_scalar(out=acc, in0=xr[:, 0:S], scalar1=cwT[:, ch, 0:1], scalar2=None,
                                                              op0=ALU.mult), B("p1_xr", "p1_cwT"), B("p1_acc"))
                        for jj in range(1, 4):
                            T.op("dve", lambda e, jj=jj: e.scalar_tensor_tensor(out=acc, in0=xr[:, jj:jj + S], scalar=cwT[:, ch, jj:jj + 1],
                                                                                in1=acc, op0=ALU.mult, op1=ALU.add),
                                 B("p1_xr", "p1_cwT", "p1_acc"), B("p1_acc"))
                        T.op("act", lambda e: e.activation(out=cvt, in_=acc, func=AF.Silu, bias=cbT[:, ch:ch + 1]),
                             B("p1_acc", "p1_cbT"), B(CVN))

                    def st2(m):
                        cvt, CVN = cvs[m % 2]
                        if n < 12:
                            tj = tpc[0] % 2
                            tpc[0] += 1
                            TP = "p1_tp%d" % tj
                            for i in range(NT):
                                T.op("pe", lambda e, i=i: e.transpose(out=tp[tj][:, i, :], in_=cvt[:, i * 128:(i + 1) * 128],
                                                                      identity=self.ident_b), B(CVN, "ident_b"), (), B(TP))
                            T.op("dve", lambda e: e.tensor_copy(out=stage[:, :, m * 128:(m + 1) * 128], in_=tp[tj]),
                                 B(TP), (), B("p1_stage"))
                        if n >= 10:
                            g = ((n - 10) % 2) * 4 + m
                            dst, dn = (self.BT_d, "BT_d") if n < 12 else (self.CT_d, "CT_d")
                            T.dma("sp", dst[b, g], cvt, *B("%s_%d" % (dn, m % 2), CVN))

                    st1(0)
                    for m in range(4):
                        if m + 1 < 4:
                            st1(m + 1)
                        st2(m)
                    if n < 12:
                        dst, dn = (self.xs_d, "xs_d") if n < 10 else (self.bt_d, "bt_d")
                        T.dma("sp", dst[b].rearrange("(i p) c -> p i c", p=128)[:, :, hh * 512:(hh + 1) * 512], stage,
                              *B(dn, "p1_stage"))
                else:
                    M = mm[0]
                    for i in range(NT):
                        for kc in range(16):
                            T.op("pe", lambda e, kc=kc, i=i: e.matmul(M[:, i * 16:(i + 1) * 16], lhsT=hT[:, kc, i * 128:(i + 1) * 128], rhs=w[:, kc, 0:16],
                                                                      start=(kc == 0), stop=(kc == 15)),
                                 B("p1_hT%d" % i, WN), (), B("p1_mm0"))
                    dflat = dts.rearrange("p i h -> p (i h)")
                    T.op("dve", lambda e: e.tensor_tensor(out=dts, in0=M[:, 0:NT * 16].rearrange("p (i h) -> p i h", h=16),
                                                          in1=dtb.unsqueeze(1).to_broadcast([128, NT, 16]), op=ALU.add),
                         B("p1_mm0", "p1_dtb"), B("p1_dts"))
                    T.op("act", lambda e: e.activation(out=dflat, in_=dflat, func=AF.Exp), B("p1_dts"), B("p1_dts"))
                    T.op("act", lambda e: e.activation(out=dflat, in_=dflat, func=AF.Ln, bias=1.0), B("p1_dts"), B("p1_dts"))
                    T.dma("sp", self.dt_d[b].rearrange("(i p) h -> p i h", p=128), dts, *B("dt_d", "p1_dts"))
            T.barrier()

    def p2(self):
        nc, T, B = self.nc, self.T, self.B
        with contextlib.ExitStack() as es:
            sb, ps = (lambda *a, **k: self.sb(es, *a, **k)), (lambda *a, **k: self.ps(es, *a, **k))
            tbx = sb("p2_tbx", [128, 19 * 128], BF16)
            ones = sb("p2_ones", [128, 128], BF16)
            V = sb("p2_V", [128, NT, 1024], BF16)
            qT = [sb("p2_qT%d" % j, [128, S], BF16) for j in range(2)]
            kT = [sb("p2_kT%d" % j, [128, S], BF16) for j in range(2)]
            P = [sb("p2_P%d" % j, [128, 512], BF16) for j in range(4)]
            rden = sb("p2_rden", [128, 512])
            yT = [sb("p2_yT%d" % j, [128, S], BF16) for j in range(2)]
            S_ps = [ps("p2_S%d" % j, [128, 512]) for j in range(4)]
            O_ps = [ps("p2_O%d" % j, [128, 512]) for j in range(2)]
            D_ps = [ps("p2_D%d" % j, [128, 512]) for j in range(2)]
            T.dma("pool", tbx, self.c_tbx, *B("p2_tbx", "c_tbx"))
            T.op("dve", lambda e: e.memset(ones, 1.0), (), B("p2_ones"))
            scale = 1.0 / math.sqrt(128.0)
            it = 0
            cc = 0
            pj = 0

            def load_qk(b, h, j):
                T.dma("sp", qT[j], self.qT_d[b, h], *B("p2_qT%d" % j, "qT_d"), partial=False)
                T.dma("sp", kT[j], self.kT_d[b, h], *B("p2_kT%d" % j, "kT_d"), partial=False)

            for b in range(NB):
                T.dma("sp", V, self.v_d[b].rearrange("(i p) c -> p i c", p=128), *B("p2_V", "v_d"), partial=False)
                load_qk(b, 0, it % 2)
                for h in range(8):
                    j = it % 2
                    it += 1
                    if h + 1 < 8:
                        load_qk(b, h + 1, it % 2)
                    QN, KN, YN = "p2_qT%d" % j, "p2_kT%d" % j, "p2_yT%d" % j
                    units = [(c, kb) for c in range(4) for kb in range(4 * c + 4)]
                    sbuf_of = {}

                    def emit_S(u):
                        c, kb = units[u]
                        sj = (pj0 + u) % 4
                        sbuf_of[u] = sj
                        SN = "p2_S%d" % sj
                        T.op("pe", lambda e: e.matmul(S_ps[sj], lhsT=kT[j][:, kb * 128:(kb + 1) * 128],
                                                      rhs=qT[j][:, c * 512:(c + 1) * 512], start=True, stop=False),
                             B(KN, QN), B(SN))
                        i0 = (4 * c - kb + 3) * 128
                        T.op("pe", lambda e: e.matmul(S_ps[sj], lhsT=self.ident_b, rhs=tbx[:, i0:i0 + 512],
                                                      start=False, stop=True), B("ident_b", "p2_tbx"), (), B(SN))

                    pj0 = pj
                    LOOK = 2
                    for u in range(min(LOOK, len(units))):
                        emit_S(u)
                    for u, (c, kb) in enumerate(units):
                        if u + LOOK < len(units):
                            emit_S(u + LOOK)
                        if kb == 0:
                            oj = cc % 2
                            cc += 1
                        ON, DN = "p2_O%d" % oj, "p2_D%d" % oj
                        nkb = 4 * c + 4
                        sj = sbuf_of[u]
                        SN, PN = "p2_S%d" % sj, "p2_P%d" % sj
                        T.op("act", lambda e: e.activation(out=P[sj], in_=S_ps[sj], func=AF.Exp, scale=scale),
                             B(SN), B(PN))
                        T.op("pe", lambda e: e.matmul(O_ps[oj], lhsT=V[:, kb, h * 128:(h + 1) * 128], rhs=P[sj],
                                                      start=(kb == 0), stop=(kb == nkb - 1)),
                             B("p2_V", PN), (), B(ON))
                        T.op("pe", lambda e: e.matmul(D_ps[oj], lhsT=ones, rhs=P[sj],
                                                      start=(kb == 0), stop=(kb == nkb - 1)),
                             B("p2_ones", PN), (), B(DN))
                        if kb == nkb - 1:
                            T.op("dve", lambda e: e.reciprocal(out=rden, in_=D_ps[oj]), B(DN), B("p2_rden"))
                            T.op("dve", lambda e: e.tensor_tensor(out=yT[j][:, c * 512:(c + 1) * 512], in0=O_ps[oj], in1=rden,
                                                                  op=ALU.mult), B(ON, "p2_rden"), (), B(YN))
                    pj += len(units)
                    T.dma("sp", self.yT_d[b, h], yT[j], *B("yT_d", YN))
            T.barrier()

    def p3(self):
        nc, T, B = self.nc, self.T, self.B
        with contextlib.ExitStack() as es:
            sb, ps = (lambda *a, **k: self.sb(es, *a, **k)), (lambda *a, **k: self.ps(es, *a, **k))
            U = sb("p3_U", [128, 128])
            onesf = sb("p3_onesf", [128, 128])
            SBm = sb("p3_SBm", [128, 4, 128], BF16)
            a_bc = sb("p3_a", [128, 16])
            dsk = sb("p3_dsk", [128, 16])
            nw = sb("p3_nw", [128, 1024])
            state = sb("p3_state", [128, 16, 64])
            state_b = sb("p3_stateb", [128, 16, 64], BF16)
            yTs = sb("p3_yTs", [128, 8, S], BF16)
            xs = [sb("p3_xs%d" % j, [128, 16, 64], BF16) for j in range(2)]
            bt = [sb("p3_bt%d" % j, [128, 1024], BF16) for j in range(2)]
            BT = [sb("p3_BT%d" % j, [128, 8, 128], BF16) for j in range(2)]
            CT = [sb("p3_CT%d" % j, [128, 8, 128], BF16) for j in range(2)]
            zc = [sb("p3_z%d" % j, [128, 1024], BF16) for j in range(2)]
            dtc = [sb("p3_dt%d" % j, [128, 16]) for j in range(2)]
            dta = sb("p3_dta", [128, 16])
            acs = sb("p3_acs", [128, 16])
            nacs = sb("p3_nacs", [128, 16])
            eacs = sb("p3_eacs", [128, 16])
            last = sb("p3_last", [128, 16])
            dte = sb("p3_dte", [128, 16])
            cd = sb("p3_cd", [128, 16])
            rhsbig = sb("p3_rhsbig", [128, 16, 128])
            dec = sb("p3_dec", [128, 8, 128])
            GT = sb("p3_GT", [128, 8, 128], BF16)
            xdt = sb("p3_xdt", [128, 16, 64], BF16)
            xdtd = sb("p3_xdtd", [128, 16, 64], BF16)
            y1 = sb("p3_y1", [128, 16, 64])
            tmp = sb("p3_tmp", [128, 16, 64])
            sz = sb("p3_sz", [128, 1024])
            s8 = sb("p3_s8", [128, 16])
            y4 = sb("p3_y4", [128, 1024], BF16)
            acs_ps = ps("p3_acsps", [128, 16])
            acsb = ps("p3_acsb", [128, 8, 128])
            cb = ps("p3_cb", [128, 4, 128])
            y_ps = ps("p3_yps", [128, 512])
            yo_ps = ps("p3_yops", [128, 512])
            Sc_ps = ps("p3_Scps", [128, 512])
            tp = ps("p3_tp", [128, 8, 128], BF16)
            T.dma("sp", U, self.c_U, *B("p3_U", "c_U"))
            for q in range(4):
                T.dma("pool", SBm[:, q, :], self.c_SB, *B("p3_SBm", "c_SB"))
            T.op("dve", lambda e: e.memset(onesf, 1.0), (), B("p3_onesf"))
            T.dma("sp", a_bc, self.alog.partition_broadcast(128), *B("p3_a", "alog"))
            T.dma("sp", dsk, self.dskip.partition_broadcast(128), *B("p3_dsk", "dskip"))
            T.dma("sp", nw, self.ssm_nw.partition_broadcast(128), *B("p3_nw", "ssm_nw"))
            T.op("act", lambda e: e.activation(out=a_bc, in_=a_bc, func=AF.Exp), B("p3_a"), B("p3_a"))
            T.op("dve", lambda e: e.tensor_scalar(out=a_bc, in0=a_bc, scalar1=-1.0, scalar2=None, op0=ALU.mult), B("p3_a"), B("p3_a"))

            def load(b, i, j):
                r0 = i * 128
                T.dma("sp", xs[j].rearrange("p h e -> p (h e)"), self.xs_d[b, r0:r0 + 128, :], *B("p3_xs%d" % j, "xs_d"), partial=False)
                T.dma("sp", bt[j], self.bt_d[b, r0:r0 + 128, :], *B("p3_bt%d" % j, "bt_d"), partial=False)
                T.dma("sp", BT[j], self.BT_d[b, :, :, r0:r0 + 128].rearrange("g n t -> n g t"), *B("p3_BT%d" % j, "BT_d"), partial=False)
                T.dma("sp", CT[j], self.CT_d[b, :, :, r0:r0 + 128].rearrange("g n t -> n g t"), *B("p3_CT%d" % j, "CT_d"), partial=False)
                T.dma("sp", zc[j], self.z_d[b, r0:r0 + 128, :], *B("p3_z%d" % j, "z_d"), partial=False)
                T.dma("sp", dtc[j], self.dt_d[b, r0:r0 + 128, :], *B("p3_dt%d" % j, "dt_d"), partial=False)

            it = 0
            for b in range(NB):
                T.op("dve", lambda e: e.memset(state, 0.0), (), B("p3_state"))
                T.op("dve", lambda e: e.memset(state_b, 0.0), (), B("p3_stateb"))
                load(b, 0, it % 2)
                for i in range(NT):
                    j = it % 2
                    it += 1
                    if i + 1 < NT:
                        load(b, i + 1, it % 2)
                    T.wait_bufs("pool", B("p3_dta"))
                    self.convert_some(2)
                    XS, BTN, BTT, CTT, ZN, DTN = ("p3_xs%d" % j, "p3_bt%d" % j, "p3_BT%d" % j, "p3_CT%d" % j,
                                                  "p3_z%d" % j, "p3_dt%d" % j)
                    sh = [128, 16, 64]
                    T.op("dve", lambda e: e.tensor_tensor(out=dta, in0=dtc[j], in1=a_bc, op=ALU.mult), B(DTN, "p3_a"), B("p3_dta"))
                    T.op("pe", lambda e: e.matmul(acs_ps, lhsT=U, rhs=dta, start=True, stop=True), B("p3_U", "p3_dta"), B("p3_acsps"))
                    T.op("dve", lambda e: e.tensor_copy(out=acs, in_=acs_ps), B("p3_acsps"), B("p3_acs"))
                    T.op("dve", lambda e: e.tensor_scalar(out=nacs, in0=acs_ps, scalar1=-1.0, scalar2=None, op0=ALU.mult),
                         B("p3_acsps"), B("p3_nacs"))
                    T.op("act", lambda e: e.activation(out=eacs, in_=acs, func=AF.Exp), B("p3_acs"), B("p3_eacs"))
                    T.op("dve", lambda e: e.tensor_tensor(out=rhsbig, in0=U.unsqueeze(1).to_broadcast([128, 16, 128]),
                                                          in1=dta.unsqueeze(2).to_broadcast([128, 16, 128]), op=ALU.mult),
                         B("p3_U", "p3_dta"), B("p3_rhsbig"))
                    T.op("dve", lambda e: e.tensor_tensor(out=xdt, in0=xs[j], in1=dtc[j].unsqueeze(2).to_broadcast(sh), op=ALU.mult),
                         B(XS, DTN), B("p3_xdt"))
                    for hh in range(2):
                        h0 = hh * 8
                        for q in range(2):
                            T.op("pe", lambda e: e.matmul(acsb[:, q * 4:(q + 1) * 4, :].rearrange("p h l -> p (h l)"), lhsT=onesf,
                                                          rhs=rhsbig[:, h0 + q * 4:h0 + (q + 1) * 4, :].rearrange("p h l -> p (h l)"),
                                                          start=True, stop=False), B("p3_onesf", "p3_rhsbig"), (), B("p3_acsb"))
                            T.op("pe", lambda e: e.matmul(acsb[:, q * 4:(q + 1) * 4, :].rearrange("p h l -> p (h l)"), lhsT=self.ident_b,
                                                          rhs=SBm.rearrange("p h l -> p (h l)"), start=False, stop=True),
                                 B("ident_b", "p3_SBm"), (), B("p3_acsb"))
                        T.op("dve", lambda e: e.tensor_copy(out=last[:, h0:h0 + 8], in_=acsb[:, :, 127]), B("p3_acsb"), (), B("p3_last"))
                        for hl in range(8):
                            h = h0 + hl
                            T.op("act", lambda e, h=h, hl=hl: e.activation(out=dec[:, hl, :], in_=acsb[:, hl, :], func=AF.Exp,
                                                                           bias=nacs[:, h:h + 1]),
                                 B("p3_acsb", "p3_nacs"), (), B("p3_dec"))
                        for gl in range(4):
                            g = hh * 4 + gl
                            T.op("pe", lambda e, g=g, gl=gl: e.matmul(cb[:, gl, :], lhsT=BT[j][:, g, :], rhs=CT[j][:, g, :],
                                                                      start=True, stop=True), B(BTT, CTT), (), B("p3_cb"))
                        T.op("dve", lambda e: e.tensor_tensor(out=GT.rearrange("p (g r) l -> p g r l", r=2),
                                                              in0=cb.unsqueeze(2).to_broadcast([128, 4, 2, 128]),
                                                              in1=dec.rearrange("p (g r) l -> p g r l", r=2), op=ALU.mult),
                             B("p3_cb", "p3_dec"), B("p3_GT"))
                        for hl in range(8):
                            h = h0 + hl
                            T.op("pe", lambda e, h=h, hl=hl: e.matmul(y_ps[:, hl * 64:(hl + 1) * 64], lhsT=GT[:, hl, :], rhs=xdt[:, h, :],
                                                                      start=True, stop=True), B("p3_GT", "p3_xdt"), (), B("p3_yps"))
                        for gl in range(4):
                            g = hh * 4 + gl
                            T.op("pe", lambda e, g=g, gl=gl: e.matmul(yo_ps[:, gl * 128:(gl + 1) * 128], lhsT=CT[j][:, g, :],
                                                                      rhs=state_b[:, 2 * g:2 * g + 2, :].rearrange("p h e -> p (h e)"),
                                                                      start=True, stop=True), B(CTT, "p3_stateb"), (), B("p3_yops"))
                        T.op("dve", lambda e: e.tensor_tensor(out=y1[:, h0:h0 + 8, :], in0=yo_ps.rearrange("p (h e) -> p h e", e=64),
                                                              in1=eacs[:, h0:h0 + 8].unsqueeze(2).to_broadcast([128, 8, 64]), op=ALU.mult),
                             B("p3_yops", "p3_eacs"), (), B("p3_y1"))
                        T.op("dve", lambda e: e.tensor_tensor(out=y1[:, h0:h0 + 8, :], in0=y1[:, h0:h0 + 8, :],
                                                              in1=y_ps.rearrange("p (h e) -> p h e", e=64), op=ALU.add),
                             B("p3_yps", "p3_y1"), (), B("p3_y1"))
                        T.op("dve", lambda e: e.tensor_tensor(out=dte[:, h0:h0 + 8], in0=last[:, h0:h0 + 8], in1=acs[:, h0:h0 + 8],
                                                              op=ALU.subtract), B("p3_last", "p3_acs"), (), B("p3_dte"))
                        T.op("act", lambda e: e.activation(out=dte[:, h0:h0 + 8], in_=dte[:, h0:h0 + 8], func=AF.Exp),
                             B("p3_dte"), (), B("p3_dte"))
                        T.op("act", lambda e: e.activation(out=cd[:, h0:h0 + 8], in_=last[:, h0:h0 + 8], func=AF.Exp),
                             B("p3_last"), (), B("p3_cd"))
                        T.op("dve", lambda e: e.tensor_tensor(out=xdtd[:, h0:h0 + 8, :], in0=xdt[:, h0:h0 + 8, :],
                                                              in1=dte[:, h0:h0 + 8].unsqueeze(2).to_broadcast([128, 8, 64]), op=ALU.mult),
                             B("p3_xdt", "p3_dte"), (), B("p3_xdtd"))
                        for gl in range(4):
                            g = hh * 4 + gl
                            T.op("pe", lambda e, g=g, gl=gl: e.matmul(Sc_ps[:, gl * 128:(gl + 1) * 128], lhsT=bt[j][:, g * 128:(g + 1) * 128],
                                                                      rhs=xdtd[:, 2 * g:2 * g + 2, :].rearrange("p h e -> p (h e)"),
                                                                      start=True, stop=True), B(BTN, "p3_xdtd"), (), B("p3_Scps"))
                        T.op("dve", lambda e: e.tensor_tensor(out=state[:, h0:h0 + 8, :], in0=state[:, h0:h0 + 8, :],
                                                              in1=cd[:, h0:h0 + 8].unsqueeze(2).to_broadcast([128, 8, 64]), op=ALU.mult),
                             B("p3_cd", "p3_state"), (), B("p3_state"))
                        T.op("dve", lambda e: e.tensor_tensor(out=state[:, h0:h0 + 8, :], in0=state[:, h0:h0 + 8, :],
                                                              in1=Sc_ps.rearrange("p (h e) -> p h e", e=64), op=ALU.add),
                             B("p3_Scps", "p3_state"), (), B("p3_state"))
                        T.op("act", lambda e: e.activation(out=state_b[:, h0:h0 + 8, :], in_=state[:, h0:h0 + 8, :], func=AF.Copy),
                             B("p3_state"), (), B("p3_stateb"))
                    T.op("dve", lambda e: e.tensor_tensor(out=tmp, in0=xs[j], in1=dsk.unsqueeze(2).to_broadcast(sh), op=ALU.mult),
                         B(XS, "p3_dsk"), B("p3_tmp"))
                    T.op("dve", lambda e: e.tensor_tensor(out=y1, in0=y1, in1=tmp, op=ALU.add), B("p3_y1", "p3_tmp"), B("p3_y1"))
                    T.op("act", lambda e: e.activation(out=sz, in_=zc[j], func=AF.Silu), B(ZN), B("p3_sz"))
                    y1f = y1.rearrange("p h e -> p (h e)")
                    T.op("dve", lambda e: e.tensor_tensor(out=y1f, in0=y1f, in1=sz, op=ALU.mult), B("p3_y1", "p3_sz"), B("p3_y1"))
                    T.op("dve", lambda e: e.tensor_tensor(out=sz, in0=y1f, in1=y1f, op=ALU.mult), B("p3_y1"), B("p3_sz"))
                    T.op("dve", lambda e: e.tensor_reduce(out=s8[:, 0:8], in_=sz.rearrange("p (g c) -> p g c", c=128), axis=AX.X, op=ALU.add),
                         B("p3_sz"), B("p3_s8"))
                    T.op("dve", lambda e: e.tensor_scalar(out=s8[:, 0:8], in0=s8[:, 0:8], scalar1=1.0 / 128, scalar2=EPS,
                                                          op0=ALU.mult, op1=ALU.add), B("p3_s8"), B("p3_s8"))
                    T.op("act", lambda e: e.activation(out=s8[:, 0:8], in_=s8[:, 0:8], func=AF.Sqrt), B("p3_s8"), B("p3_s8"))
                    T.op("dve", lambda e: e.reciprocal(out=s8[:, 8:16], in_=s8[:, 0:8]), B("p3_s8"), B("p3_s8"))
                    T.op("dve", lambda e: e.tensor_tensor(out=sz.rearrange("p (g c) -> p g c", c=128), in0=y1.rearrange("p (g r) e -> p g (r e)", r=2),
                                                          in1=s8[:, 8:16].unsqueeze(2).to_broadcast([128, 8, 128]), op=ALU.mult),
                         B("p3_y1", "p3_s8"), B("p3_sz"))
                    T.op("dve", lambda e: e.tensor_tensor(out=y4, in0=sz, in1=nw, op=ALU.mult), B("p3_sz", "p3_nw"), B("p3_y4"))
                    for g in range(8):
                        T.op("pe", lambda e, g=g: e.transpose(out=tp[:, g, :], in_=y4[:, g * 128:(g + 1) * 128], identity=self.ident_b),
                             B("p3_y4", "ident_b"), (), B("p3_tp"))
                    T.op("dve", lambda e: e.tensor_copy(out=yTs[:, :, i * 128:(i + 1) * 128], in_=tp), B("p3_tp"), (), B("p3_yTs"))
                T.dma("sp", self.yT_d[b, 8:16].rearrange("g c t -> c g t"), yTs, *B("yT_d", "p3_yTs"))
            T.barrier()

    def p4(self):
        nc, T, B = self.nc, self.T, self.B
        while self.zq:
            zo, zi = self.zq.pop()
            T.dma("sp", zo, zi, *B("xd_d", "zt"))
        with contextlib.ExitStack() as es:
            sb, ps = (lambda *a, **k: self.sb(es, *a, **k)), (lambda *a, **k: self.ps(es, *a, **k))
            wo = sb("p4_wo", [128, 16, D], BF16)
            wr = sb("p4_wr", [128, 16, 72])
            br = sb("p4_br", [128, 72])
            Ls = sb("p4_Ls", [128, 128], BF16)
            Lf = sb("p4_Lf", [128, 128])
            onesb = sb("p4_onesb", [128, 128], BF16)
            g1 = sb("p4_g1", [128, D])
            A2 = sb("p4_A2", [128, D])
            sh2 = sb("p4_sh2", [128, D])
            yTg = [sb("p4_yTg%d" % j, [128, 16, 256], BF16) for j in range(2)]
            xt = [sb("p4_xt%d" % j, [128, D]) for j in range(2)]
            x1 = [sb("p4_x1%d" % j, [128, D]) for j in range(2)]
            h2 = [sb("p4_h2%d" % j, [128, D]) for j in range(2)]
            h2b = [sb("p4_h2b%d" % j, [128, D], BF16) for j in range(2)]
            junk = sb("p4_junk", [128, D], BF16)
            h2T = sb("p4_h2T", [128, 16, 128])
            st = sb("p4_st", [128, 8])
            lg = sb("p4_lg", [128, 72])
            sm = sb("p4_sm", [128, 16])
            ohg = sb("p4_ohg", [128, 8])
            ge = sb("p4_ge", [128, 8])
            t64 = sb("p4_t64", [128, 8, 8])
            esel = sb("p4_esel", [128, 8])
            esel2 = sb("p4_esel2", [128, 8])
            oh1 = sb("p4_oh1", [128, 8])
            oh2 = sb("p4_oh2", [128, 8])
            OH1 = sb("p4_OH1", [128, 8, 8])
            OH2 = sb("p4_OH2", [128, 8, 8])
            Mf = sb("p4_Mf", [128, NE])
            Mb = sb("p4_Mb", [128, NE], BF16)
            base = sb("p4_base", [128, NE])
            rk = sb("p4_rk", [128, NE])
            dsf = sb("p4_dsf", [128, 8])
            mm = [ps("p4_mm%d" % j, [128, 512]) for j in range(4)]
            tpf = [ps("p4_tpf%d" % j, [128, 4, 128]) for j in range(2)]
            lg_ps = ps("p4_lgps", [128, 72])
            cum_ps = ps("p4_cumps", [128, 2, NE])
            T.dma("pool", wo, self.w_out.rearrange("(c p) n -> p c n", p=128), *B("p4_wo", "w_out"))
            T.dma("sp", wr, self.wr.rearrange("(c p) n -> p c n", p=128), *B("p4_wr", "wr"))
            T.dma("sp", br, self.br.partition_broadcast(128), *B("p4_br", "br"))
            T.dma("sp", Lf, self.c_U, *B("p4_Lf", "c_U"))
            T.op("dve", lambda e: e.tensor_tensor(out=Ls, in0=Lf, in1=self.ident_f, op=ALU.subtract), B("p4_Lf", "ident_f"), B("p4_Ls"))
            T.op("dve", lambda e: e.memset(onesb, 1.0), (), B("p4_onesb"))
            T.op("dve", lambda e: e.memset(self.runtot, 0.0), (), B("runtot"))
            it = 0
            for b in range(NB):
                md = self.mod_d[b]
                T.dma("sp", g1, md[2 * D:3 * D].partition_broadcast(128), *B("p4_g1", "mod_d"), partial=False)
                T.dma("sp", sh2, md[3 * D:4 * D].partition_broadcast(128), *B("p4_sh2", "mod_d"), partial=False)
                T.dma("sp", A2, md[4 * D:5 * D].partition_broadcast(128), *B("p4_A2", "mod_d"), partial=False)
                T.dma("sp", h2[0], self.n2w.partition_broadcast(128), *B("p4_h20", "n2w"), partial=False)
                T.op("dve", lambda e: e.scalar_tensor_tensor(out=A2, in0=A2, scalar=1.0, in1=h2[0], op0=ALU.add, op1=ALU.mult),
                     B("p4_A2", "p4_h20"), B("p4_A2"))

                def load_y(tg4, j):
                    T.dma("sp", yTg[j], self.yT_d[b, :, :, tg4 * 256:(tg4 + 1) * 256].rearrange("k c t -> c k t"),
                          *B("p4_yTg%d" % j, "yT_d"), partial=False)

                def load_x(i, j):
                    r0 = b * S + i * 128
                    T.dma("sp", xt[j], self.x[r0:r0 + 128, :], *B("p4_xt%d" % j, "x"), partial=False)

                load_y(0, 0)
                load_x(0, it % 2)

                def tile(i, j):
                    ti = b * NT + i
                    h2_ = h2[j]
                    H2N = "p4_h2%d" % j
                    tg4, tt = i // 2, i % 2
                    yj = tg4 % 2
                    if tt == 0 and tg4 + 1 < NT // 2:
                        load_y(tg4 + 1, (tg4 + 1) % 2)
                    if i + 1 < NT:
                        load_x(i + 1, (j + 1) % 2)
                    T.wait_bufs("pool", B("p4_st"))
                    self.convert_some(2)
                    YN, XN, X1N, HBN = "p4_yTg%d" % yj, "p4_xt%d" % j, "p4_x1%d" % j, "p4_h2b%d" % j
                    for nq in range(4):
                        for kc in range(16):
                            T.op("pe", lambda e, nq=nq, kc=kc: e.matmul(mm[nq], lhsT=yTg[yj][:, kc, tt * 128:(tt + 1) * 128],
                                                                        rhs=wo[:, kc, nq * 512:(nq + 1) * 512],
                                                                        start=(kc == 0), stop=(kc == 15)),
                                 B(YN, "p4_wo"), (), B("p4_mm%d" % nq))
                    for nq in range(4):
                        sl = slice(nq * 512, (nq + 1) * 512)
                        T.op("dve", lambda e, nq=nq, sl=sl: e.tensor_tensor(out=x1[j][:, sl], in0=mm[nq], in1=g1[:, sl], op=ALU.mult),
                             B("p4_mm%d" % nq, "p4_g1"), (), B(X1N))
                        T.op("dve", lambda e, sl=sl: e.tensor_tensor(out=x1[j][:, sl], in0=x1[j][:, sl], in1=xt[j][:, sl], op=ALU.add),
                             B(XN, X1N), (), B(X1N))
                    r0 = b * S + i * 128
                    T.dma("sp", self.x1_d[r0:r0 + 128, :], x1[j], *B("x1_d", X1N))
                    T.op("dve", lambda e: e.memset(st[:, 0:1], 0.0), (), B("p4_st"))
                    T.op("act", lambda e: e.activation(out=junk, in_=x1[j], func=AF.Square, accum_out=st[:, 0:1]),
                         B(X1N), B("p4_junk", "p4_st"))
                    T.op("dve", lambda e: e.tensor_scalar(out=st[:, 1:2], in0=st[:, 0:1], scalar1=1.0 / D, scalar2=EPS,
                                                          op0=ALU.mult, op1=ALU.add), B("p4_st"), B("p4_st"))
                    T.op("act", lambda e: e.activation(out=st[:, 2:3], in_=st[:, 1:2], func=AF.Sqrt), B("p4_st"), B("p4_st"))
                    T.op("dve", lambda e: e.reciprocal(out=st[:, 3:4], in_=st[:, 2:3]), B("p4_st"), B("p4_st"))
                    T.op("act", lambda e: e.activation(out=h2_, in_=x1[j], func=AF.Copy, scale=st[:, 3:4]), B(X1N, "p4_st"), B(H2N))
                    T.op("dve", lambda e: e.tensor_tensor(out=h2_, in0=h2_, in1=A2, op=ALU.mult), B(H2N, "p4_A2"), B(H2N))
                    T.op("dve", lambda e: e.tensor_tensor(out=h2_, in0=h2_, in1=sh2, op=ALU.add), B(H2N, "p4_sh2"), B(H2N))
                    T.op("act", lambda e: e.activation(out=h2b[j], in_=h2_, func=AF.Copy), B(H2N), B(HBN))
                    T.mark()
                    for q in range(4):
                        tj = q % 2
                        for c4 in range(4):
                            c = q * 4 + c4
                            T.op("pe", lambda e, c=c, c4=c4, tj=tj: e.transpose(out=tpf[tj][:, c4, :], in_=h2_[:, c * 128:(c + 1) * 128],
                                                                                identity=self.ident_f),
                                 B(H2N, "ident_f"), (), B("p4_tpf%d" % tj))
                        if q % 2 == 0:
                            T.op("dve", lambda e, q=q, tj=tj: e.tensor_copy(out=h2T[:, q * 4:(q + 1) * 4, :], in_=tpf[tj]),
                                 B("p4_tpf%d" % tj), (), B("p4_h2T"))
                        else:
                            T.op("act", lambda e, q=q, tj=tj: e.activation(out=h2T[:, q * 4:(q + 1) * 4, :], in_=tpf[tj], func=AF.Copy),
                                 B("p4_tpf%d" % tj), (), B("p4_h2T"))
                    for kc in range(16):
                        T.op("pe", lambda e, kc=kc: e.matmul(lg_ps, lhsT=h2T[:, kc, :], rhs=wr[:, kc, :], start=(kc == 0), stop=(kc == 15)),
                             B("p4_h2T", "p4_wr"), (), B("p4_lgps"))
                    T.op("dve", lambda e: e.tensor_tensor(out=lg, in0=lg_ps, in1=br, op=ALU.add), B("p4_lgps", "p4_br"), B("p4_lg"))
                    SM = B("p4_sm")
                    T.op("dve", lambda e: e.tensor_reduce(out=sm[:, 0:1], in_=lg[:, 0:8], axis=AX.X, op=ALU.max), B("p4_lg"), SM)
                    T.op("dve", lambda e: e.tensor_scalar(out=ohg, in0=lg[:, 0:8], scalar1=sm[:, 0:1], scalar2=None, op0=ALU.is_equal),
                         B("p4_lg", "p4_sm"), B("p4_ohg"))
                    T.op("dve", lambda e: e.tensor_scalar(out=ge, in0=lg[:, 0:8], scalar1=sm[:, 0:1], scalar2=None, op0=ALU.subtract),
                         B("p4_lg", "p4_sm"), B("p4_ge"))
                    T.op("act", lambda e: e.activation(out=ge, in_=ge, func=AF.Exp), B("p4_ge"), B("p4_ge"))
                    T.op("dve", lambda e: e.tensor_reduce(out=sm[:, 1:2], in_=ge, axis=AX.X, op=ALU.add), B("p4_ge"), SM)
                    T.op("dve", lambda e: e.reciprocal(out=sm[:, 2:3], in_=sm[:, 1:2]), SM, SM)
                    T.op("dve", lambda e: e.tensor_tensor(out=t64, in0=lg[:, 8:72].rearrange("p (g e) -> p g e", e=8),
                                                          in1=ohg.unsqueeze(2).to_broadcast([128, 8, 8]), op=ALU.mult),
                         B("p4_lg", "p4_ohg"), B("p4_t64"))
                    T.op("dve", lambda e: e.tensor_reduce(out=esel, in_=t64.rearrange("p g e -> p e g"), axis=AX.X, op=ALU.add),
                         B("p4_t64"), B("p4_esel"))
                    T.op("dve", lambda e: e.tensor_reduce(out=sm[:, 3:4], in_=esel, axis=AX.X, op=ALU.max), B("p4_esel"), SM)
                    T.op("dve", lambda e: e.tensor_scalar(out=oh1, in0=esel, scalar1=sm[:, 3:4], scalar2=None, op0=ALU.is_equal),
                         B("p4_esel", "p4_sm"), B("p4_oh1"))
                    T.op("dve", lambda e: e.scalar_tensor_tensor(out=esel2, in0=oh1, scalar=-1e30, in1=esel, op0=ALU.mult, op1=ALU.add),
                         B("p4_oh1", "p4_esel"), B("p4_esel2"))
                    T.op("dve", lambda e: e.tensor_reduce(out=sm[:, 4:5], in_=esel2, axis=AX.X, op=ALU.max), B("p4_esel2"), SM)
                    T.op("dve", lambda e: e.tensor_scalar(out=oh2, in0=esel2, scalar1=sm[:, 4:5], scalar2=None, op0=ALU.is_equal),
                         B("p4_esel2", "p4_sm"), B("p4_oh2"))
                    T.op("dve", lambda e: e.tensor_tensor(out=sm[:, 5:6], in0=sm[:, 4:5], in1=sm[:, 3:4], op=ALU.subtract), SM, SM)
                    T.op("act", lambda e: e.activation(out=sm[:, 5:6], in_=sm[:, 5:6], func=AF.Exp), SM, SM)
                    T.op("dve", lambda e: e.tensor_scalar(out=sm[:, 6:7], in0=sm[:, 5:6], scalar1=1.0, scalar2=None, op0=ALU.add), SM, SM)
                    T.op("dve", lambda e: e.reciprocal(out=sm[:, 7:8], in_=sm[:, 6:7]), SM, SM)
                    T.op("dve", lambda e: e.tensor_tensor(out=sm[:, 8:9], in0=sm[:, 5:6], in1=sm[:, 7:8], op=ALU.mult), SM, SM)
                    T.op("dve", lambda e: e.tensor_scalar(out=self.swt[:, ti, :], in0=sm[:, 7:9], scalar1=sm[:, 2:3], scalar2=None,
                                                          op0=ALU.mult), SM, (), B("swt"))
                    T.op("dve", lambda e: e.tensor_tensor(out=OH1, in0=ohg.unsqueeze(2).to_broadcast([128, 8, 8]),
                                                          in1=oh1.unsqueeze(1).to_broadcast([128, 8, 8]), op=ALU.mult),
                         B("p4_ohg", "p4_oh1"), B("p4_OH1"))
                    T.op("dve", lambda e: e.tensor_tensor(out=OH2, in0=ohg.unsqueeze(2).to_broadcast([128, 8, 8]),
                                                          in1=oh2.unsqueeze(1).to_broadcast([128, 8, 8]), op=ALU.mult),
                         B("p4_ohg", "p4_oh2"), B("p4_OH2"))
                    O1f, O2f = OH1.rearrange("p g e -> p (g e)"), OH2.rearrange("p g e -> p (g e)")
                    T.op("dve", lambda e: e.tensor_tensor(out=Mb, in0=O1f, in1=O2f, op=ALU.add), B("p4_OH1", "p4_OH2"), B("p4_Mb"))
                    T.op("pe", lambda e: e.matmul(cum_ps[:, 0, :], lhsT=Ls, rhs=Mb, start=True, stop=True), B("p4_Ls", "p4_Mb"), (), B("p4_cumps"))
                    T.op("pe", lambda e: e.matmul(cum_ps[:, 1, :], lhsT=onesb, rhs=Mb, start=True, stop=True), B("p4_onesb", "p4_Mb"), (), B("p4_cumps"))
                    T.op("dve", lambda e: e.tensor_tensor(out=rk, in0=cum_ps[:, 0, :], in1=self.runtot, op=ALU.add), B("p4_cumps", "runtot"), B("p4_rk"))
                    T.op("dve", lambda e: e.tensor_tensor(out=self.runtot, in0=self.runtot, in1=cum_ps[:, 1, :], op=ALU.add), B("p4_cumps", "runtot"), B("runtot"))
                    for kk, Of, ON in ((0, O1f, "p4_OH1"), (1, O2f, "p4_OH2")):
                        T.op("dve", lambda e, Of=Of: e.tensor_tensor(out=Mf, in0=Of, in1=rk, op=ALU.mult), B(ON, "p4_rk"), B("p4_Mf"))
                        T.op("dve", lambda e, kk=kk: e.tensor_reduce(out=self.rks[:, ti, kk:kk + 1], in_=Mf, axis=AX.X, op=ALU.add),
                             B("p4_Mf"), (), B("rks"))
                        T.op("dve", lambda e, kk=kk, Of=Of: e.tensor_copy(out=self.OHs[:, ti, kk, :], in_=Of), B(ON), (), B("OHs"))
                    T.dma("sp", self.h2_d[r0:r0 + 128, :], h2b[j], *B("h2_d", HBN))

                prev2 = []
                for i in range(NT):
                    j = it % 2
                    it += 1
                    T.record = []
                    tile(i, j)
                    rec, T.record = T.record, None
                    k = rec.index(None)
                    first, second = rec[:k], rec[k + 1:]
                    T.emit_interleaved(prev2, first)
                    prev2 = second
                T.emit_interleaved(prev2, [])
            T.barrier()

    def p4b(self):
        nc, T, B = self.nc, self.T, self.B
        self.convert_some(10 ** 6)
        with contextlib.ExitStack() as es:
            sb = lambda *a, **k: self.sb(es, *a, **k)
            bst = sb("pb_bst", [128, NBLK])
            pidx = sb("pb_pidx", [128, 1])
            f = sb("pb_f", [128, NE])
            fi = sb("pb_fi", [128, NE], I32)
            ff = sb("pb_ff", [128, NE])
            fx = sb("pb_fx", [128, NE])
            padded = sb("pb_padded", [128, NE])
            cs = [sb("pb_cs%d" % j, [128, NE]) for j in range(2)]
            pstart = sb("pb_pstart", [128, NE])
            cmp = sb("pb_cmp", [128, NBLK, NE], BF16)
            be = sb("pb_be", [128, NBLK])
            Mf = sb("pb_Mf", [128, NE])
            dsf = sb("pb_dsf", [128, 4])
            h2t = [sb("pb_h2t%d" % j, [128, D], BF16) for j in range(2)]
            T.dma("sp", bst, self.c_bst.partition_broadcast(128), *B("pb_bst", "c_bst"))
            T.dma("sp", pidx, self.c_pidx, *B("pb_pidx", "c_pidx"))
            op = lambda fn, r, w: T.op("dve", fn, B(*r), B(*w))
            op(lambda e: e.tensor_scalar(out=f, in0=self.runtot, scalar1=float(PADR - 1), scalar2=1.0 / PADR, op0=ALU.add, op1=ALU.mult), ["runtot"], ["pb_f"])
            op(lambda e: e.tensor_copy(out=fi, in_=f), ["pb_f"], ["pb_fi"])
            op(lambda e: e.tensor_copy(out=ff, in_=fi), ["pb_fi"], ["pb_ff"])
            op(lambda e: e.tensor_tensor(out=fx, in0=ff, in1=f, op=ALU.is_gt), ["pb_ff", "pb_f"], ["pb_fx"])
            op(lambda e: e.tensor_tensor(out=ff, in0=ff, in1=fx, op=ALU.subtract), ["pb_ff", "pb_fx"], ["pb_ff"])
            op(lambda e: e.tensor_scalar(out=padded, in0=ff, scalar1=float(PADR), scalar2=None, op0=ALU.mult), ["pb_ff"], ["pb_padded"])
            op(lambda e: e.tensor_copy(out=cs[0], in_=padded), ["pb_padded"], ["pb_cs0"])
            cur = 0
            for sft in (1, 2, 4, 8, 16, 32):
                a_, b_ = cs[cur], cs[1 - cur]
                an, bn = "pb_cs%d" % cur, "pb_cs%d" % (1 - cur)
                op(lambda e, a_=a_, b_=b_, sft=sft: e.tensor_tensor(out=b_[:, sft:NE], in0=a_[:, sft:NE], in1=a_[:, 0:NE - sft], op=ALU.add), [an], [bn])
                T.op("dve", lambda e, a_=a_, b_=b_, sft=sft: e.tensor_copy(out=b_[:, 0:sft], in_=a_[:, 0:sft]), B(an), (), B(bn))
                cur = 1 - cur
            pend = cs[cur]
            PEND = "pb_cs%d" % cur
            op(lambda e: e.tensor_tensor(out=pstart, in0=pend, in1=padded, op=ALU.subtract), [PEND, "pb_padded"], ["pb_pstart"])
            op(lambda e: e.tensor_tensor(out=cmp, in0=pend.unsqueeze(1).to_broadcast([128, NBLK, NE]),
                                         in1=bst.unsqueeze(2).to_broadcast([128, NBLK, NE]), op=ALU.is_le), [PEND, "pb_bst"], ["pb_cmp"])
            op(lambda e: e.tensor_reduce(out=be, in_=cmp, axis=AX.X, op=ALU.add), ["pb_cmp"], ["pb_be"])
            op(lambda e: e.tensor_scalar(out=be, in0=be, scalar1=float(NE - 1), scalar2=128.0, op0=ALU.min, op1=ALU.mult), ["pb_be"], ["pb_be"])
            op(lambda e: e.tensor_scalar(out=be, in0=be, scalar1=pidx[:, 0:1], scalar2=None, op0=ALU.add), ["pb_be", "pb_pidx"], ["pb_be"])
            op(lambda e: e.tensor_copy(out=self.widx, in_=be), ["pb_be"], ["widx"])
            T.dma("sp", h2t[0], self.h2_d[0:128, :], *B("pb_h2t0", "h2_d"), partial=False)
            for ti in range(NB * NT):
                j = ti % 2
                if ti + 1 < NB * NT:
                    T.dma("sp", h2t[1 - j], self.h2_d[(ti + 1) * 128:(ti + 2) * 128, :], *B("pb_h2t%d" % (1 - j), "h2_d"), partial=False)
                for kk in range(2):
                    op(lambda e, kk=kk: e.tensor_tensor(out=Mf, in0=self.OHs[:, ti, kk, :], in1=pstart, op=ALU.mult), ["OHs", "pb_pstart"], ["pb_Mf"])
                    T.op("dve", lambda e, kk=kk: e.tensor_reduce(out=dsf[:, kk:kk + 1], in_=Mf, axis=AX.X, op=ALU.add), B("pb_Mf"), (), B("pb_dsf"))
                op(lambda e: e.tensor_tensor(out=dsf[:, 2:4], in0=dsf[:, 0:2], in1=self.rks[:, ti, :], op=ALU.add), ["pb_dsf", "rks"], ["pb_dsf"])
                T.op("dve", lambda e: e.tensor_copy(out=self.dest[:, ti, :], in_=dsf[:, 2:4]), B("pb_dsf"), (), B("dest"))
                for kk in range(2):
                    T.dma("pool", self.xd_d, h2t[j], T.buf("xd_d"), T.buf("pb_h2t%d" % j),
                          indirect={"out": bass.IndirectOffsetOnAxis(ap=self.dest[:, ti, kk:kk + 1], axis=0)}, extra_reads=B("dest"))
            T.barrier()

    def p5(self):
        nc, T, B = self.nc, self.T, self.B
        with contextlib.ExitStack() as es:
            sb, ps = (lambda *a, **k: self.sb(es, *a, **k)), (lambda *a, **k: self.ps(es, *a, **k))
            NWB = 3
            wg = [sb("p5_wg%d" % j, [128, 16, DE], BF16) for j in range(NWB)]
            wu = [sb("p5_wu%d" % j, [128, 16, DE], BF16) for j in range(NWB)]
            wd = [sb("p5_wd%d" % j, [128, 4, D], BF16) for j in range(NWB)]
            xg = [sb("p5_xg%d" % j, [128, D], BF16) for j in range(2)]
            xgT = [sb("p5_xgT%d" % j, [128, 16, 128], BF16) for j in range(2)]
            sg = sb("p5_sg", [128, DE])
            hid = sb("p5_hid", [128, DE], BF16)
            hT = sb("p5_hT", [128, 4, 128], BF16)
            yo = [sb("p5_yo%d" % j, [128, D], BF16) for j in range(2)]
            tp = ps("p5_tp", [128, 16, 128], BF16)
            g_ps = ps("p5_g", [128, DE])
            u_ps = ps("p5_u", [128, DE])
            tph = ps("p5_tph", [128, 4, 128], BF16)
            d_ps = [ps("p5_d%d" % j, [128, 512]) for j in range(2)]

            gq = []

            def load_w(blk, j):
                off = bass.IndirectOffsetOnAxis(ap=self.widx[:, blk:blk + 1], axis=0)
                for wt_, src, nm, sn in ((wg, self.wgb_d, "p5_wg%d" % j, "wgb_d"), (wu, self.wub_d, "p5_wu%d" % j, "wub_d"),
                                         (wd, self.wdb_d, "p5_wd%d" % j, "wdb_d")):
                    if len(gq) >= 2:
                        T.wait_bufs("pool", [T.buf(gq[-2])])
                    T.dma("pool", wt_[j].rearrange("p c n -> p (c n)"), src, T.buf(nm), T.buf(sn),
                          indirect={"in": off}, extra_reads=B("widx"), partial=False)
                    gq.append(nm)

            def load_x(blk, j):
                T.dma("sp", xg[j], self.xd_d[blk * 128:(blk + 1) * 128, :], *B("p5_xg%d" % j, "xd_d"), partial=False)

            spb = PADR // 128
            nblk = NSB * spb
            dcc = [0]

            def stage_A(blk):
                j = blk % 2
                XG, XT = "p5_xg%d" % j, "p5_xgT%d" % j
                for c in range(16):
                    T.op("pe", lambda e, c=c: e.transpose(out=tp[:, c, :], in_=xg[j][:, c * 128:(c + 1) * 128], identity=self.ident_b),
                         B(XG, "ident_b"), (), B("p5_tp"))
                for q in range(4):
                    T.op("dve", lambda e, q=q: e.tensor_copy(out=xgT[j][:, q * 4:(q + 1) * 4, :], in_=tp[:, q * 4:(q + 1) * 4, :]),
                         B("p5_tp"), (), B(XT))

            def stage_Bmm(blk):
                j = blk % 2
                wj = (blk // spb) % NWB
                XT = "p5_xgT%d" % j
                for kc in range(16):
                    T.op("pe", lambda e, kc=kc: e.matmul(g_ps, lhsT=xgT[j][:, kc, :], rhs=wg[wj][:, kc, :], start=(kc == 0), stop=(kc == 15)),
                         B(XT, "p5_wg%d" % wj), (), B("p5_g"))
                for kc in range(16):
                    T.op("pe", lambda e, kc=kc: e.matmul(u_ps, lhsT=xgT[j][:, kc, :], rhs=wu[wj][:, kc, :], start=(kc == 0), stop=(kc == 15)),
                         B(XT, "p5_wu%d" % wj), (), B("p5_u"))
                T.op("act", lambda e: e.activation(out=sg, in_=g_ps, func=AF.Silu), B("p5_g"), B("p5_sg"))
                T.op("dve", lambda e: e.tensor_tensor(out=hid, in0=sg, in1=u_ps, op=ALU.mult), B("p5_sg", "p5_u"), B("p5_hid"))

            def stage_Bt(blk):
                for c in range(4):
                    T.op("pe", lambda e, c=c: e.transpose(out=tph[:, c, :], in_=hid[:, c * 128:(c + 1) * 128], identity=self.ident_b),
                         B("p5_hid", "ident_b"), (), B("p5_tph"))
                T.op("dve", lambda e: e.tensor_copy(out=hT, in_=tph), B("p5_tph"), B("p5_hT"))

            def stage_C(blk):
                j = blk % 2
                wj = (blk // spb) % NWB
                YO = "p5_yo%d" % j
                for nq in range(4):
                    dj = dcc[0] % 2
                    dcc[0] += 1
                    for kc in range(4):
                        T.op("pe", lambda e, kc=kc, nq=nq, dj=dj: e.matmul(d_ps[dj], lhsT=hT[:, kc, :], rhs=wd[wj][:, kc, nq * 512:(nq + 1) * 512],
                                                                           start=(kc == 0), stop=(kc == 3)),
                             B("p5_hT", "p5_wd%d" % wj), (), B("p5_d%d" % dj))
                    if nq % 2 == 0:
                        T.op("act", lambda e, nq=nq, dj=dj: e.activation(out=yo[j][:, nq * 512:(nq + 1) * 512], in_=d_ps[dj], func=AF.Copy),
                             B("p5_d%d" % dj), (), B(YO))
                    else:
                        T.op("dve", lambda e, nq=nq, dj=dj: e.tensor_copy(out=yo[j][:, nq * 512:(nq + 1) * 512], in_=d_ps[dj]),
                             B("p5_d%d" % dj), (), B(YO))
                T.dma("sp", self.yd_d[blk * 128:(blk + 1) * 128, :], yo[j], *B("yd_d", YO))

            load_w(0, 0)
            load_w(1, 1)
            load_x(0, 0)
            load_x(1, 1)
            stage_A(0)
            for blk in range(nblk):
                sbi = blk // spb
                if blk + 1 < nblk:
                    stage_A(blk + 1)
                if blk + 2 < nblk:
                    load_x(blk + 2, blk % 2)
                stage_Bmm(blk)
                if blk >= 1:
                    stage_C(blk - 1)
                if blk % spb == 0 and sbi + 2 < NSB:
                    load_w(sbi + 2, (sbi + 2) % NWB)
                stage_Bt(blk)
            stage_C(nblk - 1)
            T.barrier()

    def p6(self):
        nc, T, B = self.nc, self.T, self.B
        with contextlib.ExitStack() as es:
            sb = lambda *a, **k: self.sb(es, *a, **k)
            g2 = sb("p6_g2", [128, D])
            x1 = [sb("p6_x1%d" % j, [128, D]) for j in range(2)]
            ya = [sb("p6_ya%d" % j, [128, D], BF16) for j in range(2)]
            yb = [sb("p6_yb%d" % j, [128, D], BF16) for j in range(2)]
            acc = [sb("p6_acc%d" % j, [128, D]) for j in range(2)]

            def load(ti, j):
                T.dma("sp", x1[j], self.x1_d[ti * 128:(ti + 1) * 128, :], *B("p6_x1%d" % j, "x1_d"), partial=False)
                for kk, yy, nm in ((0, ya, "p6_ya%d" % j), (1, yb, "p6_yb%d" % j)):
                    T.dma("pool", yy[j], self.yd_d, T.buf(nm), T.buf("yd_d"),
                          indirect={"in": bass.IndirectOffsetOnAxis(ap=self.dest[:, ti, kk:kk + 1], axis=0)},
                          extra_reads=B("dest"), partial=False)

            load(0, 0)
            for ti in range(NB * NT):
                j = ti % 2
                b = ti // NT
                if ti % NT == 0:
                    T.dma("sp", g2, self.mod_d[b][5 * D:6 * D].partition_broadcast(128), *B("p6_g2", "mod_d"), partial=False)
                if ti + 1 < NB * NT:
                    load(ti + 1, (ti + 1) % 2)
                AN = "p6_acc%d" % j
                T.op("act", lambda e: e.activation(out=acc[j], in_=ya[j], func=AF.Copy, scale=self.swt[:, ti, 0:1]),
                     B("p6_ya%d" % j, "swt"), B(AN))
                T.op("dve", lambda e: e.scalar_tensor_tensor(out=acc[j], in0=yb[j], scalar=self.swt[:, ti, 1:2], in1=acc[j],
                                                             op0=ALU.mult, op1=ALU.add), B("p6_yb%d" % j, "swt", AN), B(AN))
                T.op("dve", lambda e: e.tensor_tensor(out=acc[j], in0=acc[j], in1=g2, op=ALU.mult), B(AN, "p6_g2"), B(AN))
                T.op("dve", lambda e: e.tensor_tensor(out=acc[j], in0=acc[j], in1=x1[j], op=ALU.add), B(AN, "p6_x1%d" % j), B(AN))
                T.dma("sp", self.out[ti * 128:(ti + 1) * 128, :], acc[j], *B("out", AN))
            T.barrier()

    def finish(self):
        T = self.T
        if "mod" in self.dumps:
            self.dump("mod", self.mod_d, T.buf("mod_d"), [NB, 6 * D])
        if "route" in self.dumps:
            self.dump("widx", self.widx, T.buf("widx"), [128, NBLK], I32)
            self.dump("dest", self.dest, T.buf("dest"), [128, NB * NT, 2], I32)
            self.dump("runtot", self.runtot, T.buf("runtot"), [128, NE])
            self.dump("swt", self.swt, T.buf("swt"), [128, NB * NT, 2])
        if "rope" in self.dumps:
            self.dump("cos", self.cos_t, T.buf("cos_t"), [128, NB * NT, 16])
            self.dump("sin", self.sin_t, T.buf("sin_t"), [128, NB * NT, 16])
        for nm, ap, shape, dt in (("qT", self.qT_d, [NB, 8, 128, S], BF16), ("kT", self.kT_d, [NB, 8, 128, S], BF16),
                                  ("v", self.v_d, [NB, S, 1024], BF16), ("z", self.z_d, [NB, S, 1024], BF16),
                                  ("xs", self.xs_d, [NB, S, 1024], BF16), ("bt", self.bt_d, [NB, S, 1024], BF16),
                                  ("BT", self.BT_d, [NB, 8, 128, S], BF16), ("CT", self.CT_d, [NB, 8, 128, S], BF16),
                                  ("dt", self.dt_d, [NB, S, 16], F32), ("yT", self.yT_d, [NB, 16, 128, S], BF16),
                                  ("x1", self.x1_d, [NB * S, D], F32)):
            if nm in self.dumps:
                self.dump(nm, ap, T.buf(nm + "_d"), shape, dt)
        T.barrier(final=True)


def host_consts():
    c = {}
    c["c_ident"] = np.eye(128, dtype=np.float32)
    ps_ = np.arange(128)[:, None]
    col = np.arange(19 * 128)[None, :]
    delta = col // 128 - 3
    pt = col % 128
    Dm = 128 * delta + pt - ps_
    Wm = ((Dm >= 0) & (Dm <= 128)).astype(np.float64) + ((Dm >= 0) & (Dm <= 512) & (Dm % 4 == 0)) \
        + ((Dm >= 0) & (Dm <= 2048) & (Dm % 16 == 0))
    with np.errstate(divide="ignore"):
        tb = np.where(Wm > 0, np.log(np.maximum(Wm, 1e-30)) * math.sqrt(128.0), NEG)
    c["c_tbx"] = tb.astype(np.float32)
    k = np.arange(128)
    c["c_U"] = (k[:, None] <= k[None, :]).astype(np.float32)
    c["c_SB"] = np.where(k[None, :] >= k[:, None], 0.0, NEG).astype(np.float32)
    c["c_invf"] = (500000.0 ** (-(np.arange(0, 32, 2, dtype=np.float32) / 32.0))).astype(np.float32)
    c["c_bst"] = (np.arange(NSB) * PADR).astype(np.float32)
    c["c_pidx"] = np.arange(128, dtype=np.float32).reshape(128, 1)
    return c


def host_shared(inp):
    f = lambda a: np.ascontiguousarray(a, dtype=np.float32)
    sh = {}
    sh["ada_w"] = f(inp["ada_w"][0])
    sh["ada_bT"] = f(inp["ada_b"][0].reshape(96, 128).T)
    sh["n1wT"] = f(inp["norm1_w"][0].reshape(16, 128).T)
    sh["w_in"] = f(inp["w_in"][0])
    sh["qkw"] = f(np.stack([inp["q_norm_w"][0], inp["k_norm_w"][0]]))
    sh["cwT"] = f(inp["conv_w"][0].reshape(4, 24, 128).transpose(2, 1, 0))
    sh["cbT"] = f(inp["conv_b"][0].reshape(24, 128).T)
    sh["dtb"] = f(inp["dt_bias"][0])
    sh["alog"] = f(inp["a_log"][0])
    sh["dskip"] = f(inp["d_skip"][0])
    sh["ssm_nw"] = f(inp["ssm_norm_w"][0])
    sh["w_out"] = f(inp["w_out"][0])
    sh["n2w"] = f(inp["norm2_w"][0])
    sh["wr"] = f(np.concatenate([inp["router_group_w"][0], inp["router_expert_w"][0]], axis=1))
    sh["br"] = f(np.concatenate([inp["router_group_b"][0], inp["router_expert_b"][0]]))
    sh["w_gate"] = f(inp["w_gate"][0].reshape(NE, 16, 128, DE).transpose(0, 2, 1, 3).reshape(NE * 128, 16 * DE))
    sh["w_up"] = f(inp["w_up"][0].reshape(NE, 16, 128, DE).transpose(0, 2, 1, 3).reshape(NE * 128, 16 * DE))
    sh["w_down"] = f(inp["w_down"][0].reshape(NE, 4, 128, D).transpose(0, 2, 1, 3).reshape(NE * 128, 4 * D))
    sh.update(host_consts())
    return sh


def host_core(inp, core):
    b0 = core * NB
    m = {}
    m["x"] = np.ascontiguousarray(inp["x"][b0:b0 + NB].reshape(NB * S, D), dtype=np.float32)
    c2 = np.asarray(inp["c"][b0:b0 + NB], dtype=np.float32)
    m["cT"] = np.ascontiguousarray(c2.reshape(NB, 16, 128).transpose(2, 1, 0))
    p2 = np.asarray(inp["positions"][b0:b0 + NB], dtype=np.int32)
    m["pos"] = np.ascontiguousarray(p2.reshape(NB * NT, 128).T)
    return m


def build(stop_after=None, dumps=()):
    k = K(stop_after=stop_after)
    k.declare()
    k.dumps = set(dumps)
    phases = [("p0", k.p0)]
    for nm in ("p1", "p2", "p3", "p4", "p4b", "p5", "p6"):
        if hasattr(k, nm):
            phases.append((nm, getattr(k, nm)))
    for nm, fn in phases:
        fn()
        if stop_after is not None and stop_after == nm or (stop_after or "").startswith("p1") and nm == "p1":
            break
    k.finish()
    return k


def kernel(**inputs):
    k = build()
    sh = host_shared(inputs)
    in_maps = []
    for c in range(NCORES):
        m = dict(sh)
        m.update(host_core(inputs, c))
        in_maps.append(m)
    res = run_bass_kernel_spmd(k.nc, in_maps, core_ids=list(range(NCORES)))
    outs = [np.asarray(r["out"]).reshape(NB, S, D) for r in res.results]
    return np.concatenate(outs, axis=0).astype(np.float32)
```
